# Optimizing a Trainium2 kernel written in Bass

```python
import math
import jax, jax.numpy as jnp
from jax import lax
import numpy as np

D_MODEL = 1024
BATCH = 8
SEQ = 4096
DEPTH = 1

CONV_DIM = 512
CONV_K = 3
N_HEADS = 8
N_KV_HEADS = 2
HEAD_DIM = 64
Q_DIM = N_HEADS * HEAD_DIM
KV_DIM = N_KV_HEADS * HEAD_DIM
GROUP = N_HEADS // N_KV_HEADS
WINDOW = 128
BLOCK = 128
N_BRANCHES = 2
N_BUCKETS = 32
MAX_DISTANCE = 128
PEER_HEADS = 8
N_KEYS = 128
N_EXPERTS = N_KEYS * N_KEYS
PEER_QDIM = 256
PEER_HALF = PEER_QDIM // 2
PEER_TOPK = 16
PEER_CHUNK = 128
EPS = 1e-6
IN_DIM = 3 * CONV_DIM + Q_DIM + 2 * KV_DIM + N_BRANCHES * D_MODEL

kernel_name = "hybrid_conv_swa_sink_peer_block"


def rms_norm(x, g):
    xf = x.astype(jnp.float32)
    y = xf * lax.rsqrt(jnp.mean(xf * xf, axis=-1, keepdims=True) + EPS)
    return (y * g.astype(jnp.float32)).astype(x.dtype)


def t5_causal_bucket(dist):
    max_exact = N_BUCKETS // 2
    d = jnp.maximum(dist, 0)
    df = jnp.maximum(d, 1).astype(jnp.float32)
    large = max_exact + (jnp.log(df / max_exact) / math.log(MAX_DISTANCE / max_exact)
                         * (N_BUCKETS - max_exact)).astype(jnp.int32)
    large = jnp.minimum(large, N_BUCKETS - 1)
    return jnp.where(d < max_exact, d, large)


def band_bias_and_mask(rel_bias, n_blocks):
    q_loc = jnp.arange(BLOCK, dtype=jnp.int32)[:, None]
    k_loc = jnp.arange(2 * BLOCK, dtype=jnp.int32)[None, :]
    dist = q_loc + BLOCK - k_loc
    bias = jnp.take(rel_bias, t5_causal_bucket(dist), axis=0)
    bias = jnp.transpose(bias, (2, 0, 1)).reshape(N_KV_HEADS, GROUP, 1, BLOCK, 2 * BLOCK)
    blk = jnp.arange(n_blocks, dtype=jnp.int32)[:, None, None]
    k_pos = blk * BLOCK - BLOCK + k_loc[None]
    valid = (dist[None] >= 0) & (dist[None] < WINDOW) & (k_pos >= 0)
    return bias.astype(jnp.float32), valid


def short_gated_conv(u, b_gate, c_gate, w_conv):
    h = c_gate * u
    kern = w_conv[:, None, :].astype(h.dtype)
    y = lax.conv_general_dilated(h, kern, window_strides=(1,), padding=[(CONV_K - 1, 0)],
                                 dimension_numbers=("NWC", "WIO", "NWC"),
                                 feature_group_count=CONV_DIM)
    return b_gate * y


def sliding_window_attention(q, k, v, sinks, bias, valid):
    bsz, s = q.shape[0], q.shape[1]
    nb = s // BLOCK
    qb = q.reshape(bsz, nb, BLOCK, N_KV_HEADS, GROUP, HEAD_DIM)
    kp = jnp.pad(k, ((0, 0), (BLOCK, 0), (0, 0), (0, 0)))
    vp = jnp.pad(v, ((0, 0), (BLOCK, 0), (0, 0), (0, 0)))
    kb = jnp.concatenate([kp[:, :s].reshape(bsz, nb, BLOCK, N_KV_HEADS, HEAD_DIM),
                          k.reshape(bsz, nb, BLOCK, N_KV_HEADS, HEAD_DIM)], axis=2)
    vb = jnp.concatenate([vp[:, :s].reshape(bsz, nb, BLOCK, N_KV_HEADS, HEAD_DIM),
                          v.reshape(bsz, nb, BLOCK, N_KV_HEADS, HEAD_DIM)], axis=2)
    logits = jnp.einsum("bnqkgd,bnskd->bkgnqs", qb, kb).astype(jnp.float32)
    logits = jnp.where(valid, logits + bias, -jnp.inf)
    sink = sinks.astype(jnp.float32).reshape(N_KV_HEADS, GROUP, 1, 1, 1)
    m = jnp.maximum(jnp.max(logits, axis=-1, keepdims=True), sink)
    p = jnp.exp(logits - m)
    denom = jnp.sum(p, axis=-1, keepdims=True) + jnp.exp(sink - m)
    p = (p / denom).astype(v.dtype)
    out = jnp.einsum("bkgnqs,bnskd->bnqkgd", p, vb)
    return out.reshape(bsz, s, Q_DIM)


def peer_ffn(x, w_query, sub_keys, expert_u, expert_v):
    bsz, s, d = x.shape
    t = bsz * s
    xt = x.reshape(t, d)
    q = (xt @ w_query).reshape(t, PEER_HEADS, 2, PEER_HALF)
    scores = jnp.einsum("thpd,hpnd->thpn", q, sub_keys).astype(jnp.float32)
    s_half, i_half = lax.top_k(scores, PEER_TOPK)
    cand_s = s_half[:, :, 0, :, None] + s_half[:, :, 1, None, :]
    cand_i = i_half[:, :, 0, :, None] * N_KEYS + i_half[:, :, 1, None, :]
    top_s, top_pos = lax.top_k(cand_s.reshape(t, PEER_HEADS, PEER_TOPK * PEER_TOPK), PEER_TOPK)
    idx = jnp.take_along_axis(cand_i.reshape(t, PEER_HEADS, PEER_TOPK * PEER_TOPK), top_pos, axis=-1)
    gates = jax.nn.softmax(top_s, axis=-1).astype(x.dtype)
    nc = t // PEER_CHUNK
    n_sel = PEER_HEADS * PEER_TOPK

    def chunk_fn(args):
        xc, ic, gc = args
        u = jnp.take(expert_u, ic, axis=0)
        a = jax.nn.gelu(jnp.einsum("cd,ced->ce", xc, u), approximate=False)
        vv = jnp.take(expert_v, ic, axis=0)
        return jnp.einsum("ce,ced->cd", (gc * a).astype(vv.dtype), vv)

    out = lax.map(chunk_fn, (xt.reshape(nc, PEER_CHUNK, d),
                             idx.reshape(nc, PEER_CHUNK, n_sel),
                             gates.reshape(nc, PEER_CHUNK, n_sel)))
    return out.reshape(bsz, s, d)


def setup_inputs(seed: int = 0) -> dict:
    key = jax.random.key(seed)
    ks = jax.random.split(key, 18)
    f32 = jnp.float32
    nrm = lambda k, shape, scale: jax.random.normal(k, shape, f32) * scale
    return {
        "x": nrm(ks[0], (BATCH, SEQ, D_MODEL), 1.0),
        "norm_mix": 1.0 + nrm(ks[1], (DEPTH, D_MODEL), 0.02),
        "norm_ffn": 1.0 + nrm(ks[2], (DEPTH, D_MODEL), 0.02),
        "w_in": nrm(ks[3], (DEPTH, D_MODEL, IN_DIM), D_MODEL ** -0.5),
        "b_gate": nrm(ks[4], (DEPTH, N_BRANCHES * D_MODEL), 0.02),
        "w_conv": nrm(ks[5], (DEPTH, CONV_K, CONV_DIM), CONV_K ** -0.5),
        "q_norm": 1.0 + nrm(ks[6], (DEPTH, HEAD_DIM), 0.02),
        "k_norm": 1.0 + nrm(ks[7], (DEPTH, HEAD_DIM), 0.02),
        "sinks": nrm(ks[8], (DEPTH, N_HEADS), 0.5),
        "rel_bias": nrm(ks[9], (N_BUCKETS, N_HEADS), 0.5),
        "w_conv_out": nrm(ks[10], (DEPTH, CONV_DIM, D_MODEL), CONV_DIM ** -0.5),
        "w_attn_out": nrm(ks[11], (DEPTH, Q_DIM, D_MODEL), Q_DIM ** -0.5),
        "w_out": nrm(ks[12], (DEPTH, D_MODEL, D_MODEL), D_MODEL ** -0.5),
        "w_query": nrm(ks[13], (DEPTH, D_MODEL, PEER_HEADS * PEER_QDIM), D_MODEL ** -0.5),
        "sub_keys": nrm(ks[14], (DEPTH, PEER_HEADS, 2, N_KEYS, PEER_HALF), PEER_HALF ** -0.5),
        "expert_u": nrm(ks[15], (DEPTH, N_EXPERTS, D_MODEL), D_MODEL ** -0.5),
        "expert_v": nrm(ks[16], (DEPTH, N_EXPERTS, D_MODEL), PEER_HEADS ** -0.5),
    }


def reference(x, norm_mix, norm_ffn, w_in, b_gate, w_conv, q_norm, k_norm, sinks, rel_bias,
              w_conv_out, w_attn_out, w_out, w_query, sub_keys, expert_u, expert_v):
    bsz, s, _ = x.shape
    bias, valid = band_bias_and_mask(rel_bias, s // BLOCK)
    offs = np.cumsum([CONV_DIM, CONV_DIM, CONV_DIM, Q_DIM, KV_DIM, KV_DIM, D_MODEL]).tolist()
    for l in range(DEPTH):
        h = rms_norm(x, norm_mix[l])
        proj = h @ w_in[l]
        u_c, b_c, c_c, q, k, v, g_c, g_a = jnp.split(proj, offs, axis=-1)
        bg_c, bg_a = jnp.split(b_gate[l], [D_MODEL])
        gate_conv = jax.nn.sigmoid(g_c + bg_c)
        gate_attn = jax.nn.sigmoid(g_a + bg_a)
        y_conv = short_gated_conv(u_c, b_c, c_c, w_conv[l]) @ w_conv_out[l]
        qh = rms_norm(q.reshape(bsz, s, N_HEADS, HEAD_DIM), q_norm[l]) * (HEAD_DIM ** -0.5)
        kh = rms_norm(k.reshape(bsz, s, N_KV_HEADS, HEAD_DIM), k_norm[l])
        vh = v.reshape(bsz, s, N_KV_HEADS, HEAD_DIM)
        y_attn = sliding_window_attention(qh, kh, vh, sinks[l], bias, valid) @ w_attn_out[l]
        mixed = gate_conv * y_conv + gate_attn * y_attn
        x = x + mixed @ w_out[l]
        h2 = rms_norm(x, norm_ffn[l])
        x = x + peer_ffn(h2, w_query[l], sub_keys[l], expert_u[l], expert_v[l])
    return x
```

```python
import numpy as np
from contextlib import ExitStack
import concourse.bass as bass
import concourse.mybir as mybir
from concourse.bass_utils import run_bass_kernel_spmd

F32 = mybir.dt.float32
BF16 = mybir.dt.bfloat16
I32 = mybir.dt.int32
U32 = mybir.dt.uint32
AF = mybir.ActivationFunctionType
ALU = mybir.AluOpType
AX = mybir.AxisListType

ENG_NAMES = ["pe", "act", "dve", "pool", "sp"]


class TT:
    def __init__(self, h, name, kind="sb"):
        self.h = h
        self.name = name
        self.kind = kind
        self.trk = self
        self.wev = {}
        self.rev = {}
        self.dsem = None
        self.dcnt = 0

    def __getitem__(self, idx):
        return self.h[idx]


class TTV(TT):
    def __init__(self, base, name, c0, n):
        TT.__init__(self, base.h, name, base.kind)
        self.trk = base.trk
        self.c0 = c0
        self.n = n

    def __getitem__(self, idx):
        if not isinstance(idx, tuple):
            idx = (idx, slice(None))
        r, c = idx
        start = 0 if c.start is None else c.start
        stop = self.n if c.stop is None else c.stop
        return self.h[r, self.c0 + start:self.c0 + stop]


def _freeze(fn):
    import types
    if fn.__closure__ is None:
        return fn
    cells = []
    for c in fn.__closure__:
        try:
            cells.append(types.CellType(c.cell_contents))
        except ValueError:
            cells.append(c)
    return types.FunctionType(fn.__code__, fn.__globals__, fn.__name__, fn.__defaults__, tuple(cells))


def _merge(d, src, skip=None):
    for k, (s, v) in src.items():
        if k == skip:
            continue
        if k not in d or d[k][1] < v:
            d[k] = (s, v)


class Sched:
    def __init__(self, nc, es):
        self.nc = nc
        self.es = es
        self.prog = {e: [] for e in ENG_NAMES}
        self.sem = {e: es.enter_context(nc.semaphore("s_" + e)) for e in ENG_NAMES}
        self.cnt = {e: 0 for e in ENG_NAMES}
        self.seen = {e: {} for e in ENG_NAMES}
        self.pend = {e: ([], []) for e in ENG_NAMES}
        self.final = {}
        self.ninst = 0
        self.nwait = 0
        self.ndsem = 0
        self.tts = []

    def _reg(self, t):
        self.tts.append(t)
        return t

    def sb(self, name, shape, dt, es=None):
        es = es or self.es
        return self._reg(TT(es.enter_context(self.nc.sbuf_tensor(name, list(shape), dt)), name))

    def ps(self, name, shape, dt=F32, es=None):
        es = es or self.es
        return self._reg(TT(es.enter_context(self.nc.psum_tensor(name, list(shape), dt)), name, "ps"))

    def dr(self, name, shape, dt, kind="Internal"):
        return self._reg(TT(self.nc.dram_tensor(name, list(shape), dt, kind=kind).ap(), name, "dr"))

    def alias(self, t, name):
        return self._reg(TT(t.h, name, t.kind))

    def _wait(self, eng, deps):
        for k, (s, v) in deps.items():
            if self.seen[eng].get(k, 0) < v:
                self.prog[eng].append(("w", s, v))
                self.seen[eng][k] = v
                self.nwait += 1

    def op(self, eng, fn, r=(), w=(), inc=True):
        w = [t.trk for t in w] + [t.trk for t in r if t.kind == "ps"]
        r = [t.trk for t in r if t.kind != "ps"]
        deps = {}
        for t in r:
            _merge(deps, t.wev)
        for t in w:
            _merge(deps, t.wev)
            _merge(deps, t.rev)
        if eng == "pe":
            deps.pop("s_pe", None)
        self._wait(eng, deps)
        self.ninst += 1
        pr, pw = self.pend[eng]
        pr.extend(r)
        pw.extend(w)
        self.prog[eng].append(("o", _freeze(fn), inc))
        if inc:
            self.cnt[eng] += 1
            k = "s_" + eng
            ev = (self.sem[eng], self.cnt[eng])
            for t in pw:
                t.wev = {k: ev}
                t.rev = {}
            for t in pr:
                if t not in pw:
                    t.rev[k] = ev
            self.pend[eng] = ([], [])

    def dma(self, q, dst, dst_ap, src, src_ap, final=False, nowaw=False, **kw):
        if dst is not None and dst.kind == "sb":
            owner = dst
        elif src is not None and src.kind == "sb":
            owner = src
        else:
            owner = dst if dst is not None else src
        if owner.dsem is None:
            owner.dsem = self.es.enter_context(self.nc.semaphore("d_" + owner.name))
            self.ndsem += 1
        k = "d_" + owner.name
        deps = {}
        if src is not None:
            _merge(deps, src.wev)
        if dst is not None:
            if not nowaw:
                _merge(deps, dst.wev, skip=k)
            _merge(deps, dst.rev)
        self._wait(q, deps)
        owner.dcnt += 16
        ev = (owner.dsem, owner.dcnt)
        self.prog[q].append(("d", lambda e: e.dma_start(out=dst_ap, in_=src_ap, **kw), owner.dsem))
        self.ninst += 1
        if dst is not None:
            dst.wev[k] = ev
            dst.rev = {}
        if src is not None:
            src.rev[k] = ev
        if final:
            self.final[k] = ev

    def emit(self):
        nc = self.nc
        for e in ENG_NAMES:
            assert not self.pend[e][0] and not self.pend[e][1], "unresolved pending on " + e
        for k, (s, v) in self.final.items():
            self.prog["sp"].append(("w", s, v))

        def replay(name, e):
            sem = self.sem[name]
            for it in self.prog[name]:
                if it[0] == "w":
                    e.wait_ge(it[1], it[2])
                elif it[0] == "o":
                    ins = it[1](e)
                    if it[2]:
                        ins.then_inc(sem, 1)
                else:
                    it[1](e).then_inc(it[2], 16)

        with nc.Block() as block:
            @block.tensor
            def _(e):
                replay("pe", e)

            @block.scalar
            def _(e):
                replay("act", e)

            @block.vector
            def _(e):
                replay("dve", e)

            @block.gpsimd
            def _(e):
                replay("pool", e)

            @block.sync
            def _(e):
                replay("sp", e)


def make_ident(S, ident, tmp):
    S.op("pool", lambda e: e.iota(out=tmp[:], pattern=[[1, 128]], base=0, channel_multiplier=-1), w=[tmp])
    S.op("dve", lambda e: e.tensor_single_scalar(out=ident[:], in_=tmp[:], scalar=0.0, op=ALU.is_equal), r=[tmp], w=[ident])


def cap(tt, offset, dims):
    t = tt[:]
    return bass.AP(t.tensor, offset, [[t.ap[0][0], t.ap[0][1]]] + [list(d) for d in dims])


EPS = 1e-6
HEADS_PERM = [0, 4, 1, 5, 2, 6, 3, 7]
NGRP = 64
CPG = 2
NSET = 4
R_RING = 4


def t5_bucket_np(d):
    d = np.maximum(d, 0)
    df = np.maximum(d, 1).astype(np.float32)
    large = 16 + (np.log(df / 16) / np.log(128 / 16) * 16).astype(np.int32)
    large = np.minimum(large, 31)
    return np.where(d < 16, d, large)


class Barrier:
    pass


def sched_barrier(S):
    tts = S.tts
    deps = {}
    for e in ENG_NAMES:
        if S.cnt[e] > 0:
            deps["s_" + e] = (S.sem[e], S.cnt[e])
    for t in tts:
        if t.dsem is not None and t.dcnt > 0:
            deps["d_" + t.name] = (t.dsem, t.dcnt)
    for e in ENG_NAMES:
        assert not S.pend[e][0] and not S.pend[e][1]
        S._wait(e, dict(deps))


def build_program(SEQ_T=4096, dbg=False):
    import os
    KSTOP = int(os.environ.get("KSTOP", "99"))
    KSUB = int(os.environ.get("KSUB", "99"))
    nc = bass.Bass("TRN2", target_bir_lowering=False)
    NB = SEQ_T // 128
    NT = SEQ_T // 256
    DI = lambda n, shp, dt=F32: nc.dram_tensor(n, list(shp), dt, kind="ExternalInput").ap()
    x_d = DI("x", [SEQ_T, 1024])
    win_d = DI("w_in", [1024, 4352])
    wco_d = DI("w_co", [512, 1024])
    wao_d = DI("w_ao", [512, 1024])
    wout_d = DI("w_out", [1024, 1024])
    wq_d = DI("w_q", [1024, 2048])
    skT_d = DI("skT", [128, 2048])
    utg_d = DI("utg", [NGRP, 128, 2048])
    vg_d = DI("vg", [NGRP, 128, 2048])
    gmix_d = DI("gmix", [128, 1024])
    gffn_d = DI("gffn", [128, 1024])
    bgate_d = DI("bgate", [128, 16])
    wconv_d = DI("wconv", [128, 12])
    qkn_d = DI("qkn", [128, 2])
    sink_d = DI("sinkr", [128, 8])
    relb_d = DI("relb", [32, 8])
    ohb_d = DI("ohb", [32, 384])
    mask8_d = DI("mask8", [8, 384])
    y_d = nc.dram_tensor("y", [SEQ_T, 1024], F32, kind="ExternalOutput").ap()
    dbg = bool(int(os.environ.get("KDBG", "0")))
    dbg_d = {}
    if dbg:
        for n_, w_ in (("sc", 2048), ("st1", 256), ("it1f", 256), ("ts", 128), ("pif", 128), ("pjf", 128), ("idx1f", 128), ("idx2f", 128), ("gat", 128),
                       ("idx1T", 256), ("gatT", 256), ("G0", 256), ("G5", 256), ("h2T0", 256), ("ge", 256)):
            dbg_d[n_] = nc.dram_tensor("dbg_" + n_, [128, w_], F32, kind="ExternalOutput").ap()
        for n_, w_ in (("h2Tb", 2048), ("qTsb", 4096), ("skTb", 2048), ("wqb", 2048)):
            dbg_d[n_] = nc.dram_tensor("dbg_" + n_, [128, w_], BF16, kind="ExternalOutput").ap()

    with ExitStack() as es:
        S = Sched(nc, es)
        x1_s = S.dr("x1_s", [SEQ_T, 1024], F32)
        es_s = S.dr("es_s", [NGRP, 128, 4096], BF16)
        r8_s = S.dr("r8_s", [8, 128, 384], F32)

        ident_bf = S.sb("ident_bf", [128, 128], BF16)
        ident_f = S.sb("ident_f", [128, 128], F32)
        iota_f = S.sb("iota_f", [128, 128], F32)
        gffn = S.sb("gffn_sb", [128, 1024], F32)
        eps_t = S.sb("eps_t", [128, 1], F32)
        S.op("dve", lambda e: e.memset(eps_t[:], EPS), w=[eps_t])

        with ExitStack() as e1:
            NSTG = 2
            cst_f = [S.sb("cst_f%d" % i, [128, 1024], F32, es=e1) for i in range(NSTG)]
            cst_b = [S.sb("cst_b%d" % i, [128, 1024], BF16, es=e1) for i in range(NSTG)]
            tmpi = S.sb("tmpi", [128, 128], I32, es=e1)
            make_ident(S, ident_bf, tmpi)
            S.op("dve", lambda e: e.tensor_single_scalar(out=ident_f[:], in_=tmpi[:], scalar=0.0, op=ALU.is_equal), r=[tmpi], w=[ident_f])
            tmpj = S.sb("tmpj", [128, 128], I32, es=e1)
            S.op("pool", lambda e: e.iota(out=tmpj[:], pattern=[[1, 128]], base=0, channel_multiplier=0), w=[tmpj])
            S.op("dve", lambda e: e.tensor_copy(out=iota_f[:], in_=tmpj[:]), r=[tmpj], w=[iota_f])
            blockones = S.sb("blockones", [128, 128], BF16, es=e1)
            S.op("dve", lambda e: e.memset(blockones[:], 0.0), w=[blockones])
            S.op("dve", lambda e: e.memset(blockones[0:64, 0:64], 1.0), w=[blockones])
            S.op("dve", lambda e: e.memset(blockones[64:128, 64:128], 1.0), w=[blockones])

            gmix = S.sb("gmix_sb", [128, 1024], F32, es=e1)
            bgate = S.sb("bgate_sb", [128, 16], F32, es=e1)
            wconv = S.sb("wconv_sb", [128, 12], F32, es=e1)
            qkn = S.sb("qkn_sb", [128, 2], F32, es=e1)
            gq = S.sb("gq_sb", [128, 1], F32, es=e1)
            negh1 = S.sb("negh1", [128, 1], F32, es=e1)
            S.op("dve", lambda e: e.memset(negh1[:], -0.5), w=[negh1])
            nbgate = S.sb("nbgate_sb", [128, 16], F32, es=e1)
            sinkr = S.sb("sinkr_sb", [128, 8], F32, es=e1)
            esink = S.sb("esink_sb", [128, 8], F32, es=e1)
            relb = S.sb("relb_sb", [32, 8], F32, es=e1)
            ohb = S.sb("ohb_sb", [32, 384], F32, es=e1)
            mask8 = S.sb("mask8_sb", [8, 384], F32, es=e1)
            r8 = S.sb("r8_sb", [8, 384], F32, es=e1)
            biasT = S.sb("biasT", [128, 2, 8, 128], F32, es=e1)
            for t, d in ((gmix, gmix_d), (gffn, gffn_d), (bgate, bgate_d), (wconv, wconv_d), (qkn, qkn_d),
                         (sinkr, sink_d), (relb, relb_d), (ohb, ohb_d), (mask8, mask8_d)):
                S.dma("sp", t, t[:], None, d)
            S.op("act", lambda e: e.mul(out=gq[:], in_=qkn[:, 0:1], mul=0.125), r=[qkn], w=[gq])
            S.op("act", lambda e: e.mul(out=nbgate[:], in_=bgate[:], mul=-1.0), r=[bgate], w=[nbgate])
            S.op("act", lambda e: e.activation(out=esink[:], in_=sinkr[:], func=AF.Exp), r=[sinkr], w=[esink])

            ps_l = S.ps("ps_l", [128, 1024], F32, es=e1)
            ps_v = [S.ps("ps_v%d" % i, [128, 512], F32, es=e1) for i in range(2)]
            ps_x = S.ps("ps_x", [128, 512], F32, es=e1)
            ps_tr = S.ps("ps_tr", [128, 8, 128], BF16, es=e1)
            ps_pb = [S.ps("ps_pb%d" % i, [128, 512], F32, es=e1) for i in range(2)]
            ps_p = [S._reg(TTV(ps_pb[0], "ps_p0", 0, 256)), S._reg(TTV(ps_pb[1], "ps_p1", 0, 256)), S._reg(TTV(ps_x, "ps_p2", 0, 256)),
                    S._reg(TTV(ps_v[0], "ps_p3", 0, 256)), S._reg(TTV(ps_v[1], "ps_p4", 0, 256)),
                    S._reg(TTV(ps_pb[0], "ps_p5", 256, 256)), S._reg(TTV(ps_pb[1], "ps_p6", 256, 256)), S._reg(TTV(ps_x, "ps_p7", 256, 256))]
            pp_i = [0]

            def next_pp():
                t = ps_p[pp_i[0] % len(ps_p)]
                pp_i[0] += 1
                return t

            S.op("pe", lambda e: e.matmul(ps_x[0:8, 0:384], lhsT=relb[:], rhs=ohb[:], start=True, stop=True), r=[relb, ohb], w=[ps_x])
            S.op("dve", lambda e: e.tensor_tensor(out=r8[:], in0=ps_x[0:8, 0:384], in1=mask8[:], op=ALU.add), r=[ps_x, mask8], w=[r8])
            r8a = r8[:]
            S.dma("sp", r8_s, r8_s[:], r8, bass.AP(r8a.tensor, 0, [[r8a.ap[0][0], 8], [0, 128], [1, 384]]))
            r8t = r8_s[:].tensor
            for part, off in ((0, 255), (1, 127)):
                src = bass.AP(r8t, off, [[383, 128], [128 * 384, 8], [1, 128]])
                S.dma("sp", biasT, biasT[:, part, :, :], r8_s, src)

            w_in = S.sb("w_in_bf", [128, 8, 4352], BF16, es=e1)
            wco = S.sb("wco_bf", [128, 4, 1024], BF16, es=e1)
            wao = S.sb("wao_bf", [128, 4, 1024], BF16, es=e1)
            wout = S.sb("wout_bf", [128, 8, 1024], BF16, es=e1)
            with ExitStack() as e0:
                stg = [S.sb("stg%d" % i, [128, 2176], F32, es=e0) for i in range(6)]
                pieces = []
                for kc in range(8):
                    for hf in range(2):
                        pieces.append((win_d[kc * 128:(kc + 1) * 128, hf * 2176:(hf + 1) * 2176], w_in, w_in[:, kc, hf * 2176:(hf + 1) * 2176], 2176))
                for cc in range(4):
                    pieces.append((wco_d[cc * 128:(cc + 1) * 128, :], wco, wco[:, cc, :], 1024))
                    pieces.append((wao_d[cc * 128:(cc + 1) * 128, :], wao, wao[:, cc, :], 1024))
                for kc in range(8):
                    pieces.append((wout_d[kc * 128:(kc + 1) * 128, :], wout, wout[:, kc, :], 1024))
                for i, (src, dt_, dap, n) in enumerate(pieces):
                    sg = stg[i % 6]
                    S.dma("sp", sg, sg[:, 0:n], None, src)
                    eng = "dve" if i % 2 == 0 else "act"
                    if eng == "dve":
                        S.op("dve", lambda e, dap=dap, sap=sg[:, 0:n]: e.tensor_copy(out=dap, in_=sap), r=[sg], w=[dt_])
                    else:
                        S.op("act", lambda e, dap=dap, sap=sg[:, 0:n]: e.copy(out=dap, in_=sap), r=[sg], w=[dt_])
                sched_barrier(S)

            cast_jobs = []
            for g in range(NGRP):
                for q in range(2):
                    cast_jobs.append((utg_d[g, :, q * 1024:(q + 1) * 1024], es_s, es_s[g, :, q * 1024:(q + 1) * 1024]))
                    cast_jobs.append((vg_d[g, :, q * 1024:(q + 1) * 1024], es_s, es_s[g, :, 2048 + q * 1024:2048 + (q + 1) * 1024]))
            cast_state = {"ld": 0, "cv": 0}

            def cast_loads():
                pass

            def cast_step(n_conv):
                for _ in range(n_conv):
                    i = cast_state["cv"]
                    if i >= len(cast_jobs):
                        return
                    S.dma("pool", cast_jobs[i][1], cast_jobs[i][2], None, cast_jobs[i][0], nowaw=True)
                    cast_state["cv"] += 1

            JOBS_PER_ST = (len(cast_jobs) + NT - 1) // NT
            tick_state = {"n": 0}

            def cast_tick():
                if KSTOP < 3:
                    return
                tick_state["n"] += 1
                if tick_state["n"] <= JOBS_PER_ST:
                    cast_step(1)

            xt = [S.sb("xt%d" % i, [128, 2, 1024], F32, es=e1) for i in range(2)]
            x1t = [S.sb("x1t%d" % i, [128, 2, 1024], F32, es=e1) for i in range(1)]
            ss = S.sb("ss", [128, 4], F32, es=e1)
            sd = S.sb("sd", [128, 4], F32, es=e1)
            rstd = S.sb("rstd", [128, 4], F32, es=e1)
            xn = [S.sb("xn%d" % i, [128, 1024], BF16, es=e1) for i in range(2)]
            hTs = [S.sb("hT%d" % i, [128, 8, 256], BF16, es=e1) for i in range(2)]
            u_sb = S.sb("u_sb", [128, 256], F32, es=e1)
            hcv = [S.sb("hcv%d" % i, [128, 258], F32, es=e1) for i in range(4)]
            cacc = S.sb("cacc", [128, 256], F32, es=e1)
            ycTs = [S.sb("ycT%d" % i, [128, 4, 256], BF16, es=e1) for i in range(2)]
            sq = S.sb("sq", [128, 256], BF16, es=e1)
            sdq = S.sb("sdq", [128, 256], F32, es=e1)
            qT = S.sb("qT", [128, 4, 256], BF16, es=e1)
            kA = [S.sb("kA%d" % i, [128, 128], BF16, es=e1) for i in range(R_RING)]
            kB = [S.sb("kB%d" % i, [128, 128], BF16, es=e1) for i in range(R_RING)]
            vaug = [S.sb("vaug%d" % i, [128, 2, 65], BF16, es=e1) for i in range(R_RING)]
            lb = S.sb("lb", [128, 1024], F32, es=e1)
            pT = [S.sb("pT%d" % i, [128, 1024], BF16, es=e1) for i in range(2)]
            dn = S.sb("dn", [128, 8], F32, es=e1)
            rd = S.sb("rd", [128, 8], F32, es=e1)
            atok = S.sb("atok", [128, 512], BF16, es=e1)
            attnTs = [S.sb("attnT%d" % i, [128, 4, 256], BF16, es=e1) for i in range(2)]
            sgc = S.sb("sgc", [128, 256], F32, es=e1)
            sga = S.sb("sga", [128, 256], F32, es=e1)
            mixT = S.sb("mixT", [128, 8, 256], BF16, es=e1)

            for i in range(R_RING):
                S.op("dve", lambda e, i=i: e.memset(kA[i][:], 0.0), w=[kA[i]])
                S.op("dve", lambda e, i=i: e.memset(kB[i][:], 0.0), w=[kB[i]])
                S.op("dve", lambda e, i=i: e.memset(vaug[i][:], 1.0), w=[vaug[i]])
            for i in range(4):
                S.op("dve", lambda e, i=i: e.memset(hcv[i][:], 0.0), w=[hcv[i]])

            def proj(out_t, col0, hT):
                for kc in range(8):
                    S.op("pe", lambda e, kc=kc: e.matmul(out_t[:, 0:256], lhsT=w_in[:, kc, col0:col0 + 128], rhs=hT[:, kc, :],
                                                         start=(kc == 0), stop=(kc == 7)),
                         r=[w_in, hT], w=[out_t], inc=(kc == 7))

            def norm_T(xsrc, bi, gam, xn_t, dstT, ptr):
                S.op("act", lambda e: e.activation(out=xn_t[:], in_=xsrc[:, bi, :], func=AF.Square, accum_out=ss[:, bi:bi + 1]),
                     r=[xsrc], w=[xn_t, ss])
                S.op("dve", lambda e: e.tensor_scalar(out=sd[:, bi:bi + 1], in0=ss[:, bi:bi + 1], scalar1=1.0 / 1024, scalar2=EPS, op0=ALU.mult, op1=ALU.add), r=[ss], w=[sd])
                S.op("pool", lambda e: e.tensor_tensor(out=rstd[:, bi:bi + 1], in0=sd[:, bi:bi + 1], in1=negh1[:, 0:1], op=ALU.pow), r=[sd, negh1], w=[rstd])
                S.op("dve", lambda e: e.scalar_tensor_tensor(out=xn_t[:], in0=xsrc[:, bi, :], scalar=rstd[:, bi:bi + 1], in1=gam[:],
                                                             op0=ALU.mult, op1=ALU.mult), r=[xsrc, rstd, gam], w=[xn_t])
                for kc in range(8):
                    S.op("pe", lambda e, kc=kc: e.transpose(out=ptr[:, kc, :], in_=xn_t[:, kc * 128:(kc + 1) * 128], identity=ident_bf[:]),
                         r=[xn_t, ident_bf], w=[ptr], inc=(kc == 7))
                S.op("act", lambda e: e.copy(out=dstT[:, :, bi * 128:(bi + 1) * 128], in_=ptr[:]), r=[ptr], w=[dstT])

            EPS_AP = [eps_t]

            S.dma("sp", xt[0], xt[0][:], None, x_d[0:256, :].rearrange("(b p) d -> p b d", p=128))
            def stageA(st):
                xs = xt[st % 2]
                hTa, ycTa, attnTa = hTs[st % 2], ycTs[st % 2], attnTs[st % 2]
                for bi in range(2):
                    norm_T(xs, bi, gmix, xn[bi], hTa, ps_tr)
                    yield
                def conv_it(cc):
                    pu, pc, pb = next_pp(), next_pp(), next_pp()
                    proj(pu, cc * 128, hTa)
                    proj(pc, 1024 + cc * 128, hTa)
                    proj(pb, 512 + cc * 128, hTa)
                    hc = hcv[cc]
                    S.op("act", lambda e, pu=pu: e.copy(out=u_sb[:], in_=pu[:]), r=[pu], w=[u_sb])
                    S.op("dve", lambda e, pc=pc, hc=hc: e.tensor_tensor(out=hc[:, 2:258], in0=pc[:], in1=u_sb[:], op=ALU.mult), r=[pc, u_sb], w=[hc])
                    S.op("dve", lambda e, hc=hc, cc=cc: e.tensor_scalar(out=cacc[:], in0=hc[:, 2:258], scalar1=wconv[:, 8 + cc:9 + cc], scalar2=None, op0=ALU.mult),
                         r=[hc, wconv], w=[cacc])
                    S.op("dve", lambda e, hc=hc, cc=cc: e.scalar_tensor_tensor(out=cacc[:], in0=hc[:, 1:257], scalar=wconv[:, 4 + cc:5 + cc], in1=cacc[:],
                                                                             op0=ALU.mult, op1=ALU.add), r=[hc, wconv, cacc], w=[cacc])
                    S.op("dve", lambda e, hc=hc, cc=cc: e.scalar_tensor_tensor(out=cacc[:], in0=hc[:, 0:256], scalar=wconv[:, cc:cc + 1], in1=cacc[:],
                                                                             op0=ALU.mult, op1=ALU.add), r=[hc, wconv, cacc], w=[cacc])
                    S.op("dve", lambda e, pb=pb, cc=cc: e.tensor_tensor(out=ycTa[:, cc, :], in0=pb[:], in1=cacc[:], op=ALU.mult), r=[pb, cacc], w=[ycTa])
                    S.op("act", lambda e, hc=hc: e.copy(out=hc[:, 0:2], in_=hc[:, 256:258]), r=[hc], w=[hc])

                def qk_it(j):
                    pq = next_pp()
                    proj(pq, 1536 + j * 128, hTa)
                    S.op("act", lambda e, pq=pq: e.activation(out=sq[:], in_=pq[:], func=AF.Square), r=[pq], w=[sq])
                    pss = next_pp()
                    S.op("pe", lambda e, pss=pss: e.matmul(pss[:], lhsT=blockones[:], rhs=sq[:], start=True, stop=True), r=[blockones, sq], w=[pss])
                    S.op("act", lambda e, pss=pss: e.activation(out=sdq[:], in_=pss[:], func=AF.Ln, scale=1.0 / 64, bias=eps_t[:, 0:1]), r=[pss, eps_t], w=[sdq])
                    S.op("act", lambda e: e.activation(out=sdq[:], in_=sdq[:], func=AF.Exp, scale=-0.5), r=[sdq], w=[sdq])
                    if j < 4:
                        S.op("dve", lambda e, pq=pq, j=j: e.scalar_tensor_tensor(out=qT[:, j, :], in0=pq[:], scalar=gq[:, 0:1], in1=sdq[:],
                                                                               op0=ALU.mult, op1=ALU.mult), r=[pq, gq, sdq], w=[qT])
                    else:
                        for bi in range(2):
                            slot = (2 * st + bi) % R_RING
                            S.op("dve", lambda e, pq=pq, bi=bi, slot=slot: e.scalar_tensor_tensor(
                                out=kA[slot][0:64, :], in0=pq[0:64, bi * 128:(bi + 1) * 128], scalar=qkn[0:64, 1:2], in1=sdq[0:64, bi * 128:(bi + 1) * 128],
                                op0=ALU.mult, op1=ALU.mult), r=[pq, qkn, sdq], w=[kA[slot]])
                            S.op("dve", lambda e, pq=pq, bi=bi, slot=slot: e.scalar_tensor_tensor(
                                out=kB[slot][64:128, :], in0=pq[64:128, bi * 128:(bi + 1) * 128], scalar=qkn[64:128, 1:2], in1=sdq[64:128, bi * 128:(bi + 1) * 128],
                                op0=ALU.mult, op1=ALU.mult), r=[pq, qkn, sdq], w=[kB[slot]])


                for i5 in range(5):
                    qk_it(i5)
                    cast_tick()
                    yield
                    if i5 < 4:
                        conv_it(i5)
                        cast_tick()
                        yield
                for bi in range(2 if KSUB >= 4 else 0):
                    slot = (2 * st + bi) % R_RING
                    pv = next_pp()
                    for kc in range(8):
                        S.op("pe", lambda e, kc=kc, bi=bi, pv=pv: e.matmul(pv[:, 0:128], lhsT=hTa[:, kc, bi * 128:(bi + 1) * 128], rhs=w_in[:, kc, 2176:2304],
                                                                           start=(kc == 0), stop=(kc == 7)), r=[hTa, w_in], w=[pv], inc=(kc == 7))
                    S.op("act", lambda e, pv=pv, slot=slot: e.copy(out=vaug[slot][:, :, 0:64], in_=pv[:, 0:128].rearrange("p (k d) -> p k d", k=2)),
                         r=[pv], w=[vaug[slot]])
                for bi in range(2 if KSUB >= 5 else 0):
                    gb = 2 * st + bi
                    parts = [1] if gb == 0 else [0, 1]
                    for part in parts:
                        kslot = (gb - 1 + part) % R_RING
                        for hs in range(8):
                            kt = kA[kslot] if hs % 2 == 0 else kB[kslot]
                            S.op("pe", lambda e, kt=kt, hs=hs, bi=bi: e.matmul(ps_l[:, hs * 128:(hs + 1) * 128], lhsT=kt[:], rhs=qT[:, hs // 2, bi * 128:(bi + 1) * 128],
                                                                              start=True, stop=True), r=[kt, qT], w=[ps_l], inc=(hs == 7))
                        S.op("dve", lambda e, part=part: e.tensor_tensor(out=lb[:], in0=ps_l[:], in1=biasT[:, part, :, :].rearrange("p h t -> p (h t)"), op=ALU.add),
                             r=[ps_l, biasT], w=[lb])
                        S.op("act", lambda e, part=part: e.activation(out=pT[part][:], in_=lb[:], func=AF.Exp), r=[lb], w=[pT[part]])
                    for hs in range(8):
                        pvt = ps_v[hs // 4]
                        for pi, part in enumerate(parts):
                            kslot = (gb - 1 + part) % R_RING
                            S.op("pe", lambda e, pvt=pvt, hs=hs, part=part, kslot=kslot, pi=pi: e.matmul(
                                pvt[:, (hs % 4) * 128:(hs % 4) * 128 + 65], lhsT=pT[part][:, hs * 128:(hs + 1) * 128], rhs=vaug[kslot][:, hs % 2, :],
                                start=(pi == 0), stop=(pi == len(parts) - 1)), r=[pT[part], vaug[kslot]], w=[pvt], inc=(pi == len(parts) - 1 and hs % 4 == 3))
                    for g2 in range(2):
                        pv3 = ps_v[g2][:].rearrange("p (h c) -> p h c", h=4)
                        S.op("dve", lambda e, g2=g2, pv3=pv3: e.tensor_tensor(out=dn[:, g2 * 4:(g2 + 1) * 4], in0=pv3[:, :, 64], in1=esink[:, g2 * 4:(g2 + 1) * 4], op=ALU.add),
                             r=[ps_v[g2], esink], w=[dn])
                    S.op("dve", lambda e: e.reciprocal(out=rd[:], in_=dn[:]), r=[dn], w=[rd])
                    for g2 in range(2):
                        pv3 = ps_v[g2][:].rearrange("p (h c) -> p h c", h=4)
                        S.op("dve", lambda e, g2=g2, pv3=pv3: e.tensor_tensor(
                            out=atok[:, g2 * 256:(g2 + 1) * 256].rearrange("p (h d) -> p h d", h=4), in0=pv3[:, :, 0:64],
                            in1=rd[:, g2 * 4:(g2 + 1) * 4].unsqueeze(2).to_broadcast([128, 4, 64]), op=ALU.mult), r=[ps_v[g2], rd], w=[atok])
                    for j in range(4):
                        S.op("pe", lambda e, j=j: e.transpose(out=ps_tr[:, j, :], in_=atok[:, j * 128:(j + 1) * 128], identity=ident_bf[:]),
                             r=[atok, ident_bf], w=[ps_tr], inc=(j == 3))
                    S.op("act", lambda e, bi=bi: e.copy(out=attnTa[:, :, bi * 128:(bi + 1) * 128], in_=ps_tr[:, 0:4, :]), r=[ps_tr], w=[attnTa])
                    cast_tick()
                    yield

            def stageB(st):
                xs = xt[st % 2]
                hTb, ycTb, attnTb = hTs[st % 2], ycTs[st % 2], attnTs[st % 2]
                for dc in range(8 if KSUB >= 6 else 0):
                    pyc, pya, pgc, pga = next_pp(), next_pp(), next_pp(), next_pp()
                    for cc in range(4):
                        S.op("pe", lambda e, cc=cc, dc=dc, pyc=pyc: e.matmul(pyc[:], lhsT=wco[:, cc, dc * 128:(dc + 1) * 128], rhs=ycTb[:, cc, :], start=(cc == 0), stop=(cc == 3)),
                             r=[wco, ycTb], w=[pyc], inc=(cc == 3))
                    for cc in range(4):
                        S.op("pe", lambda e, cc=cc, dc=dc, pya=pya: e.matmul(pya[:], lhsT=wao[:, cc, dc * 128:(dc + 1) * 128], rhs=attnTb[:, cc, :], start=(cc == 0), stop=(cc == 3)),
                             r=[wao, attnTb], w=[pya], inc=(cc == 3))
                    proj(pgc, 2304 + dc * 128, hTb)
                    proj(pga, 3328 + dc * 128, hTb)
                    for pg_, sg_, col_ in ((pgc, sgc, dc), (pga, sga, 8 + dc)):
                        S.op("act", lambda e: e.activation(out=sg_[:], in_=pg_[:], func=AF.Exp, scale=-1.0, bias=nbgate[:, col_:col_ + 1]), r=[pg_, nbgate], w=[sg_])
                        S.op("act", lambda e: e.activation(out=sg_[:], in_=sg_[:], func=AF.Ln, bias=1.0), r=[sg_], w=[sg_])
                        S.op("act", lambda e: e.activation(out=sg_[:], in_=sg_[:], func=AF.Exp, scale=-1.0), r=[sg_], w=[sg_])
                    S.op("dve", lambda e, pyc=pyc: e.tensor_tensor(out=sgc[:], in0=pyc[:], in1=sgc[:], op=ALU.mult), r=[pyc, sgc], w=[sgc])
                    S.op("dve", lambda e, pya=pya: e.tensor_tensor(out=sga[:], in0=pya[:], in1=sga[:], op=ALU.mult), r=[pya, sga], w=[sga])
                    S.op("dve", lambda e, dc=dc: e.tensor_tensor(out=mixT[:, dc, :], in0=sgc[:], in1=sga[:], op=ALU.add), r=[sgc, sga], w=[mixT])
                    cast_tick()
                    yield
                xo = x1t[0]
                for bi in range(2 if KSUB >= 7 else 0):
                    for hf in range(2):
                        for kc in range(8):
                            S.op("pe", lambda e, kc=kc, bi=bi, hf=hf: e.matmul(ps_x[:], lhsT=mixT[:, kc, bi * 128:(bi + 1) * 128], rhs=wout[:, kc, hf * 512:(hf + 1) * 512],
                                                                              start=(kc == 0), stop=(kc == 7)), r=[mixT, wout], w=[ps_x], inc=(kc == 7))
                        S.op("dve", lambda e, bi=bi, hf=hf, xo=xo, xs=xs: e.tensor_tensor(out=xo[:, bi, hf * 512:(hf + 1) * 512], in0=ps_x[:], in1=xs[:, bi, hf * 512:(hf + 1) * 512], op=ALU.add),
                             r=[ps_x, xs], w=[xo])
                        yield
                S.dma("sp", x1_s, x1_s[st * 256:(st + 1) * 256, :].rearrange("(b p) d -> p b d", p=128), xo, xo[:], nowaw=True)
                if KSTOP < 4:
                    S.dma("sp", None, y_d[st * 256:(st + 1) * 256, :].rearrange("(b p) d -> p b d", p=128), xo, xo[:], final=True)

            if KSTOP >= 2:
                if KSTOP >= 3:
                    cast_loads()
                if NT > 1:
                    S.dma("sp", xt[1], xt[1][:], None, x_d[256:512, :].rearrange("(b p) d -> p b d", p=128))
                for _ in stageA(0):
                    pass
                for st in range(NT):
                    tick_state["n"] = 0
                    gB = stageB(st)
                    gA = stageA(st + 1) if st + 1 < NT else iter(())
                    aliveA = aliveB = True
                    while aliveA or aliveB:
                        if aliveB:
                            aliveB = next(gB, "END") != "END"
                        if aliveA:
                            aliveA = next(gA, "END") != "END"
                    if st + 2 < NT:
                        S.dma("sp", xt[st % 2], xt[st % 2][:], None, x_d[(st + 2) * 256:(st + 3) * 256, :].rearrange("(b p) d -> p b d", p=128))
            if KSTOP >= 3:
                cast_step(len(cast_jobs))
            sched_barrier(S)

        if KSTOP >= 4:
            build_phase2(nc, S, es, locals())
        S.emit()
    return nc


def build_phase2(nc, S, es, L):
    NT = L["NT"]
    x1_s, es_s = L["x1_s"], L["es_s"]
    ident_f, iota_f, gffn, eps_t = L["ident_f"], L["iota_f"], L["gffn"], L["eps_t"]
    wq_d, skT_d, y_d = L["wq_d"], L["skT_d"], L["y_d"]
    dbg_d = L["dbg_d"]
    with ExitStack() as e2:
        wq = S.sb("wq_bf", [128, 8, 2048], BF16, es=e2)
        skT = S.sb("skT_bf", [128, 16, 128], BF16, es=e2)
        with ExitStack() as e0:
            stg = [S.sb("stgq%d" % i, [128, 2048], F32, es=e0) for i in range(4)]
            for kc in range(9):
                sg = stg[kc % 4]
                if kc < 8:
                    S.dma("sp", sg, sg[:], None, wq_d[kc * 128:(kc + 1) * 128, :])
                    dap = wq[:, kc, :]
                    dt_ = wq
                else:
                    S.dma("sp", sg, sg[:], None, skT_d)
                    dap = skT[:].rearrange("p a b -> p (a b)")
                    dt_ = skT
                if kc % 2 == 0:
                    S.op("dve", lambda e, dap=dap, sg=sg: e.tensor_copy(out=dap, in_=sg[:]), r=[sg], w=[dt_])
                else:
                    S.op("act", lambda e, dap=dap, sg=sg: e.copy(out=dap, in_=sg[:]), r=[sg], w=[dt_])
            sched_barrier(S)

        acc = [S.ps("acc%d" % i, [128, 512], F32, es=e2) for i in range(4)]
        a_pb = [S.ps("a_pb%d" % i, [128, 512], F32, es=e2) for i in range(2)]
        a_ps = [S._reg(TTV(a_pb[i], "a_ps%d" % i, 0, 256)) for i in range(2)]
        pm = [S.ps("pm%d" % i, [128, 512], F32, es=e2) for i in range(2)]
        pm_i = [0]

        def next_pm():
            t = pm[pm_i[0] % 2]
            pm_i[0] += 1
            return t

        x1t = [S.sb("x1b%d" % i, [128, 2, 1024], F32, es=e2) for i in range(2)]
        ss = S.sb("ss2", [128, 2], F32, es=e2)
        sd = S.sb("sd2", [128, 2], F32, es=e2)
        rstd = S.sb("rstd2", [128, 2], F32, es=e2)
        negh = S.sb("negh", [128, 2], F32, es=e2)
        S.op("dve", lambda e: e.memset(negh[:], -0.5), w=[negh])
        xn = S.sb("xn2", [128, 1024], F32, es=e2)
        h2T = [S.sb("h2T%d" % i, [128, 8, 256], BF16, es=e2) for i in range(2)]
        qTs = S.sb("qTs", [128, 16, 256], BF16, es=e2)
        bigB = S.sb("bigB", [128, 2048], F32, es=e2)
        st1 = S.sb("st1", [128, 16, 16], F32, es=e2)
        it1 = S.sb("it1", [128, 16, 16], U32, es=e2)
        st1_a = [S.alias(st1, "st1_%d" % i) for i in range(16)]
        it1_a = [S.alias(it1, "it1_%d" % i) for i in range(16)]
        it1f = S.sb("it1f", [128, 16, 16], F32, es=e2)
        wk = [S.sb("wk%d" % i, [128, 128], F32, es=e2) for i in range(2)]
        cw = [S.sb("cw%d" % i, [128, 256], F32, es=e2) for i in range(2)]
        ts = S.sb("ts", [128, 8, 16], F32, es=e2)
        pos = S.sb("pos", [128, 8, 16], U32, es=e2)
        ts_a = [S.alias(ts, "ts_%d" % i) for i in range(8)]
        pos_a = [S.alias(pos, "pos_%d" % i) for i in range(8)]
        posi = S.sb("posi", [128, 8, 16], U32, es=e2)
        posj = S.sb("posj", [128, 8, 16], U32, es=e2)
        pif = S.sb("pif", [128, 8, 16], F32, es=e2)
        pjf = S.sb("pjf", [128, 8, 16], F32, es=e2)
        negm = S.sb("negm", [128, 8], F32, es=e2)
        eg = S.sb("eg", [128, 8, 16], F32, es=e2)
        sume = S.sb("sume", [128, 8], F32, es=e2)
        rse = S.sb("rse", [128, 8], F32, es=e2)
        gat = S.sb("gat", [128, 128], F32, es=e2)
        idx1f = S.sb("idx1f", [128, 128], F32, es=e2)
        idx2f = S.sb("idx2f", [128, 128], F32, es=e2)
        idx1T = S.sb("idx1T", [128, 256], F32, es=e2)
        idx2T = S.sb("idx2T", [128, 256], F32, es=e2)
        gatT = S.sb("gatT", [128, 256], F32, es=e2)
        nidx2T = S.sb("nidx2T", [128, 256], F32, es=e2)
        gatTs = S.sb("gatTs", [128, 256], F32, es=e2)
        NOH = 12
        Rt = [S.sb("Rt%d" % i, [128, 128], BF16, es=e2) for i in range(NOH)]
        Lt = [S.sb("Lt%d" % i, [128, 128], BF16, es=e2) for i in range(NOH)]
        G_sb = S.sb("G_sb", [128, 128, 256], BF16, es=e2)
        EX = [S.sb("EX%d" % i, [128, 4096], BF16, es=e2) for i in range(NSET)]
        ge = [S.sb("ge%d" % i, [128, 256], BF16, es=e2) for i in range(2)]
        GA = [S.sb("GA%d" % i, [128, 256], BF16, es=e2) for i in range(3)]
        iota_b = S.sb("iota_b", [128, 128], BF16, es=e2)
        S.op("dve", lambda e: e.tensor_copy(out=iota_b[:], in_=iota_f[:]), r=[iota_f], w=[iota_b])
        gring = [pm[0], pm[1]] + acc
        g_i = [0]

        def next_g():
            t = gring[g_i[0] % len(gring)]
            g_i[0] += 1
            return t

        def front(ti):
            xs = x1t[ti % 2]
            hT = h2T[ti % 2]
            S.dma("sp", xs, xs[:], x1_s, x1_s[ti * 256:(ti + 1) * 256, :].rearrange("(b p) d -> p b d", p=128))
            for bi in range(2):
                S.op("dve", lambda e, bi=bi: e.scalar_tensor_tensor(out=xn[:], in0=xs[:, bi, :], scalar=1.0, in1=xs[:, bi, :], op0=ALU.mult, op1=ALU.mult, accum_out=ss[:, bi:bi + 1]),
                     r=[xs], w=[xn, ss])
            S.op("dve", lambda e: e.tensor_scalar(out=sd[:], in0=ss[:], scalar1=1.0 / 1024, scalar2=EPS, op0=ALU.mult, op1=ALU.add), r=[ss], w=[sd])
            yield
            S.op("pool", lambda e: e.tensor_tensor(out=rstd[:], in0=sd[:], in1=negh[:], op=ALU.pow), r=[sd, negh], w=[rstd])
            yield
            for bi in range(2):
                S.op("dve", lambda e, bi=bi: e.scalar_tensor_tensor(out=xn[:], in0=xs[:, bi, :], scalar=rstd[:, bi:bi + 1], in1=gffn[:], op0=ALU.mult, op1=ALU.mult),
                     r=[xs, rstd, gffn], w=[xn])
                yield
                for rnd in range(2):
                    p = next_pm()
                    for k4 in range(4):
                        kc = rnd * 4 + k4
                        S.op("pe", lambda e, p=p, k4=k4, kc=kc: e.transpose(out=p[:, k4 * 128:(k4 + 1) * 128], in_=xn[:, kc * 128:(kc + 1) * 128], identity=ident_f[:]),
                             r=[xn, ident_f], w=[p], inc=(k4 == 3))
                    S.op("act", lambda e, p=p, rnd=rnd, bi=bi: e.copy(out=hT[:, rnd * 4:(rnd + 1) * 4, bi * 128:(bi + 1) * 128], in_=p[:].rearrange("p (k t) -> p k t", k=4)),
                         r=[p], w=[hT])
                    yield
            for hp in range(16):
                p = next_pm()
                for kc in range(8):
                    S.op("pe", lambda e, p=p, kc=kc, hp=hp: e.matmul(p[:, 0:256], lhsT=wq[:, kc, hp * 128:(hp + 1) * 128], rhs=hT[:, kc, :], start=(kc == 0), stop=(kc == 7)),
                         r=[wq, hT], w=[p], inc=(kc == 7))
                S.op("act", lambda e, p=p, hp=hp: e.copy(out=qTs[:, hp, :], in_=p[:, 0:256]), r=[p], w=[qTs])
                yield
            for tsub in range(2):
                for g4 in range(4):
                    p = next_pm()
                    for k4 in range(4):
                        hp = g4 * 4 + k4
                        S.op("pe", lambda e, p=p, k4=k4, hp=hp: e.matmul(p[:, k4 * 128:(k4 + 1) * 128], lhsT=qTs[:, hp, tsub * 128:(tsub + 1) * 128], rhs=skT[:, hp, :], start=True, stop=True),
                             r=[qTs, skT], w=[p], inc=(k4 == 3))
                    S.op("act", lambda e, p=p, g4=g4: e.copy(out=bigB[:, g4 * 512:(g4 + 1) * 512], in_=p[:]), r=[p], w=[bigB])
                    yield
                for hp0 in range(0, 16, 2):
                    pr = (hp0, hp0 + 1)
                    for hp in pr:
                        S.op("dve", lambda e, hp=hp: e.max(out=st1[:, hp, 0:8], in_=bigB[:, hp * 128:(hp + 1) * 128]), r=[bigB], w=[st1_a[hp]])
                    for hp in pr:
                        S.op("dve", lambda e, hp=hp: e.max_index(out=it1[:, hp, 0:8], in_max=st1[:, hp, 0:8], in_values=bigB[:, hp * 128:(hp + 1) * 128]), r=[bigB, st1_a[hp]], w=[it1_a[hp]])
                    for hp in pr:
                        w_ = wk[hp % 2]
                        S.op("dve", lambda e, hp=hp, w_=w_: e.match_replace(out=w_[:], in_to_replace=st1[:, hp, 0:8], in_values=bigB[:, hp * 128:(hp + 1) * 128], imm_value=-1e30), r=[bigB, st1_a[hp]], w=[w_])
                    for hp in pr:
                        w_ = wk[hp % 2]
                        S.op("dve", lambda e, hp=hp, w_=w_: e.max(out=st1[:, hp, 8:16], in_=w_[:]), r=[w_], w=[st1_a[hp]])
                    for hp in pr:
                        w_ = wk[hp % 2]
                        S.op("dve", lambda e, hp=hp, w_=w_: e.max_index(out=it1[:, hp, 8:16], in_max=st1[:, hp, 8:16], in_values=w_[:]), r=[w_, st1_a[hp]], w=[it1_a[hp]])
                    yield
                for q4 in range(4):
                    cand4 = cap(bigB, q4 * 512, [[256, 2], [16, 16], [1, 16]])
                    in0 = cap(st1, q4 * 64, [[32, 2], [1, 16], [0, 16]])
                    in1 = cap(st1, q4 * 64 + 16, [[32, 2], [0, 16], [1, 16]])
                    S.op("dve", lambda e: e.tensor_tensor(out=cand4, in0=in0, in1=in1, op=ALU.add), r=st1_a, w=[bigB])
                    yield
                for h0 in range(0, 8, 2):
                    pr = (h0, h0 + 1)
                    cins = {h: bigB[:, h * 256:(h + 1) * 256] for h in pr}
                    for h in pr:
                        S.op("dve", lambda e, h=h, cin=cins[h]: e.max(out=ts[:, h, 0:8], in_=cin), r=[bigB], w=[ts_a[h]])
                    for h in pr:
                        S.op("dve", lambda e, h=h, cin=cins[h]: e.max_index(out=pos[:, h, 0:8], in_max=ts[:, h, 0:8], in_values=cin), r=[bigB, ts_a[h]], w=[pos_a[h]])
                    for h in pr:
                        c_ = cw[h % 2]
                        S.op("dve", lambda e, h=h, cin=cins[h], c_=c_: e.match_replace(out=c_[:], in_to_replace=ts[:, h, 0:8], in_values=cin, imm_value=-1e30), r=[bigB, ts_a[h]], w=[c_])
                    for h in pr:
                        c_ = cw[h % 2]
                        S.op("dve", lambda e, h=h, c_=c_: e.max(out=ts[:, h, 8:16], in_=c_[:]), r=[c_], w=[ts_a[h]])
                    for h in pr:
                        c_ = cw[h % 2]
                        S.op("dve", lambda e, h=h, c_=c_: e.max_index(out=pos[:, h, 8:16], in_max=ts[:, h, 8:16], in_values=c_[:]), r=[c_, ts_a[h]], w=[pos_a[h]])
                    yield
                S.op("dve", lambda e: e.tensor_tensor(out=eg[:], in0=ts[:], in1=ts[:, :, 0:1].to_broadcast([128, 8, 16]), op=ALU.subtract), r=ts_a, w=[eg])
                yield
                S.op("dve", lambda e: e.tensor_single_scalar(out=posi[:], in_=pos[:], scalar=4, op=ALU.logical_shift_right), r=pos_a, w=[posi])
                S.op("dve", lambda e: e.tensor_single_scalar(out=posj[:], in_=pos[:], scalar=15, op=ALU.bitwise_and), r=pos_a, w=[posj])
                S.op("dve", lambda e: e.tensor_copy(out=pif[:], in_=posi[:]), r=[posi], w=[pif])
                S.op("dve", lambda e: e.tensor_copy(out=pjf[:], in_=posj[:]), r=[posj], w=[pjf])
                S.op("dve", lambda e: e.tensor_copy(out=it1f[:], in_=it1[:]), r=it1_a, w=[it1f])
                yield
                io4 = cap(iota_f, 0, [[0, 2], [0, 16], [1, 16]])
                for which, pf, dst in ((0, pif, idx1f), (1, pjf, idx2f)):
                    for q4 in range(4):
                        eq4 = cap(bigB, q4 * 512, [[256, 2], [16, 16], [1, 16]])
                        sel = cap(pf, q4 * 32, [[16, 2], [1, 16], [0, 16]])
                        itb = cap(it1f, q4 * 64 + which * 16, [[32, 2], [0, 16], [1, 16]])
                        S.op("dve", lambda e: e.tensor_tensor(out=eq4, in0=io4, in1=sel, op=ALU.is_equal), r=[iota_f, pf], w=[bigB])
                        S.op("dve", lambda e: e.tensor_tensor(out=eq4, in0=eq4, in1=itb, op=ALU.mult), r=[bigB, it1f], w=[bigB])
                        yield
                        S.op("dve", lambda e: e.tensor_reduce(out=dst[:, q4 * 32:(q4 + 1) * 32], in_=bigB[:, q4 * 512:(q4 + 1) * 512].rearrange("p (a b) -> p a b", b=16),
                                                              axis=AX.X, op=ALU.add), r=[bigB], w=[dst])
                        yield
                if dbg_d and ti == 0 and tsub == 0:
                    sched_barrier(S)
                    for n_, t_ in (("h2Tb", hT), ("qTsb", qTs), ("skTb", skT), ("st1", st1), ("it1f", it1f), ("ts", ts), ("pif", pif), ("pjf", pjf), ("idx1f", idx1f), ("idx2f", idx2f), ("gat", gat)):
                        a_ = t_[:]
                        if len(a_.shape) == 3:
                            a_ = a_.rearrange("p a b -> p (a b)")
                        S.dma("sp", None, dbg_d[n_], t_, a_, final=True)
                    S.dma("sp", None, dbg_d["wqb"], wq, wq[:, 3, :], final=True)
                S.op("act", lambda e: e.activation(out=eg[:], in_=eg[:], func=AF.Exp), r=[eg], w=[eg])
                yield
                S.op("dve", lambda e: e.tensor_reduce(out=sume[:], in_=eg[:], axis=AX.X, op=ALU.add), r=[eg], w=[sume])
                S.op("dve", lambda e: e.reciprocal(out=rse[:], in_=sume[:]), r=[sume], w=[rse])
                S.op("dve", lambda e: e.tensor_tensor(out=gat[:].rearrange("p (h k) -> p h k", h=8), in0=eg[:], in1=rse[:].unsqueeze(2).to_broadcast([128, 8, 16]), op=ALU.mult),
                     r=[eg, rse], w=[gat])
                yield
                p = next_pm()
                for i3, src in enumerate((idx1f, idx2f, gat)):
                    S.op("pe", lambda e, p=p, i3=i3, src=src: e.transpose(out=p[:, i3 * 128:(i3 + 1) * 128], in_=src[:], identity=ident_f[:]), r=[src, ident_f], w=[p], inc=(i3 == 2))
                yield
                for i3, dst in enumerate((idx1T, idx2T, gatT)):
                    S.op("act", lambda e, p=p, i3=i3, dst=dst: e.copy(out=dst[:, tsub * 128:(tsub + 1) * 128], in_=p[:, i3 * 128:(i3 + 1) * 128]), r=[p], w=[dst])
                S.op("act", lambda e, p=p: e.mul(out=nidx2T[:, tsub * 128:(tsub + 1) * 128], in_=p[:, 128:256], mul=-6.0), r=[p], w=[nidx2T])
                S.op("act", lambda e, p=p: e.mul(out=gatTs[:, tsub * 128:(tsub + 1) * 128], in_=p[:, 256:384], mul=1.0 / 1.125), r=[p], w=[gatTs])
                yield

        def back(ti):
            def gen_oh(t4):
                for k4 in range(4):
                    tok = t4 * 4 + k4
                    r_, l_ = Rt[tok % NOH], Lt[tok % NOH]
                    if tok % 2 == 1:
                        S.op("act", lambda e: e.activation(out=r_[:], in_=iota_f[:], func=AF.Derivative_Erf, scale=6.0, bias=nidx2T[:, tok:tok + 1]),
                             r=[iota_f, nidx2T], w=[r_])
                        g_ = gatTs
                    else:
                        S.op("dve", lambda e: e.tensor_scalar(out=r_[:], in0=iota_b[:], scalar1=idx2T[:, tok:tok + 1], scalar2=None, op0=ALU.is_equal),
                             r=[iota_b, idx2T], w=[r_])
                        g_ = gatT
                    S.op("dve", lambda e: e.tensor_scalar(out=l_[:], in0=iota_b[:], scalar1=idx1T[:, tok:tok + 1], scalar2=g_[:, tok:tok + 1], op0=ALU.is_equal, op1=ALU.mult),
                         r=[iota_b, idx1T, g_], w=[l_])

            def gen_mm(t4):
                p = next_g()
                for k4 in range(4):
                    tok = t4 * 4 + k4
                    r_, l_ = Rt[tok % NOH], Lt[tok % NOH]
                    S.op("pe", lambda e: e.matmul(p[:, k4 * 128:(k4 + 1) * 128], lhsT=r_[:], rhs=l_[:], start=True, stop=True), r=[r_, l_], w=[p], inc=(k4 == 3))
                src = cap(p, 0, [[1, 128], [128, 4]])
                S.op("act", lambda e: e.copy(out=G_sb[:, :, t4 * 4:(t4 + 1) * 4], in_=src), r=[p], w=[G_sb])

            for t4 in range(65):
                if t4 < 64:
                    gen_oh(t4)
                if t4 >= 1:
                    gen_mm(t4 - 1)

        def load_group(gg):
            if gg >= NT * NGRP:
                return
            s, g = gg % NSET, gg % NGRP
            S.dma("sp", EX[s], EX[s][:], es_s, es_s[g])

        def a_mm(ti, c):
            s = (ti * NGRP + c // CPG) % NSET
            ci = c % CPG
            ap_ = a_ps[c % 2]
            hT = h2T[ti % 2]
            for kc in range(8):
                S.op("pe", lambda e, kc=kc: e.matmul(ap_[:], lhsT=EX[s][:, kc * 256 + ci * 128:kc * 256 + (ci + 1) * 128], rhs=hT[:, kc, :], start=(kc == 0), stop=(kc == 7)),
                     r=[EX[s], hT], w=[ap_], inc=(kc == 7))
            g_, ga_ = ge[c % 2], GA[c % 3]
            S.op("act", lambda e: e.activation(out=g_[:], in_=ap_[:], func=AF.Gelu), r=[ap_], w=[g_])
            S.op("pool", lambda e: e.tensor_tensor(out=ga_[:], in0=g_[:], in1=G_sb[:, c, :], op=ALU.mult), r=[g_, G_sb], w=[ga_])

        def o_mm(ti, c):
            s = (ti * NGRP + c // CPG) % NSET
            ci = c % CPG
            ga_ = GA[c % 3]
            for tsub in range(2):
                for hf in range(2):
                    last = (tsub == 1 and hf == 1)
                    S.op("pe", lambda e, tsub=tsub, hf=hf: e.matmul(acc[tsub * 2 + hf][:], lhsT=ga_[:, tsub * 128:(tsub + 1) * 128], rhs=EX[s][:, 2048 + ci * 1024 + hf * 512:2048 + ci * 1024 + (hf + 1) * 512],
                                                                   start=(c == 0), stop=(c == 127)), r=[ga_, EX[s]], w=[acc[tsub * 2 + hf]], inc=last)

        LOOKAHEAD = 2
        for gg in range(NSET):
            load_group(gg)
        n_front = sum(1 for _ in front(0))
        for ti in range(NT):
            back(ti)
            fgen = front(ti + 1) if ti + 1 < NT else iter(())
            pulled = 0
            for c in range(128 + LOOKAHEAD):
                if c < 128:
                    a_mm(ti, c)
                if c >= LOOKAHEAD:
                    co = c - LOOKAHEAD
                    o_mm(ti, co)
                    if co % CPG == CPG - 1:
                        load_group(ti * NGRP + co // CPG + NSET)
                if c >= 2:
                    target = ((c - 1) * n_front + 121) // 122
                    while pulled < target:
                        next(fgen, None)
                        pulled += 1
            for _ in fgen:
                pass
            xs = x1t[ti % 2]
            for tsub in range(2):
                for hf in range(2):
                    S.op("dve", lambda e, tsub=tsub, hf=hf: e.tensor_tensor(out=xs[:, tsub, hf * 512:(hf + 1) * 512], in0=acc[tsub * 2 + hf][:], in1=xs[:, tsub, hf * 512:(hf + 1) * 512], op=ALU.add),
                         r=[acc[tsub * 2 + hf], xs], w=[xs])
            S.dma("sp", None, y_d[ti * 256:(ti + 1) * 256, :].rearrange("(b p) d -> p b d", p=128), xs, xs[:], final=True)
        sched_barrier(S)


def _host_layout(inputs):
    f = lambda a: np.ascontiguousarray(np.asarray(a, dtype=np.float32))
    qcols = np.concatenate([np.arange(h * 64, (h + 1) * 64) for h in HEADS_PERM])
    cols = np.arange(4352)
    cols[1536:2048] = 1536 + qcols
    sh = {}
    sh["w_in"] = f(np.asarray(inputs["w_in"])[0][:, cols])
    sh["w_co"] = f(np.asarray(inputs["w_conv_out"])[0])
    sh["w_ao"] = f(np.asarray(inputs["w_attn_out"])[0][qcols, :])
    sh["w_out"] = f(np.asarray(inputs["w_out"])[0])
    sh["w_q"] = f(np.asarray(inputs["w_query"])[0])
    sk = np.asarray(inputs["sub_keys"])[0]
    sh["skT"] = f(sk.transpose(3, 0, 1, 2).reshape(128, 2048))
    eu = np.asarray(inputs["expert_u"])[0]
    sh["utg"] = f(eu.T.reshape(8, 128, NGRP, CPG * 128).transpose(2, 1, 0, 3).reshape(NGRP, 128, 8 * CPG * 128))
    ev = np.asarray(inputs["expert_v"])[0]
    sh["vg"] = f(ev.reshape(NGRP, CPG, 128, 1024).transpose(0, 2, 1, 3).reshape(NGRP, 128, CPG * 1024))
    sh["gmix"] = f(np.tile(np.asarray(inputs["norm_mix"])[0][None, :], (128, 1)))
    sh["gffn"] = f(np.tile(np.asarray(inputs["norm_ffn"])[0][None, :], (128, 1)))
    sh["bgate"] = f(np.asarray(inputs["b_gate"])[0].reshape(16, 128).T)
    sh["wconv"] = f(np.asarray(inputs["w_conv"])[0].reshape(3, 4, 128).transpose(2, 0, 1).reshape(128, 12))
    qn = np.asarray(inputs["q_norm"])[0]
    kn = np.asarray(inputs["k_norm"])[0]
    sh["qkn"] = f(np.stack([np.tile(qn, 2), np.tile(kn, 2)], axis=1))
    sh["sinkr"] = f(np.tile(np.asarray(inputs["sinks"])[0][HEADS_PERM][None, :], (128, 1)))
    sh["relb"] = f(np.asarray(inputs["rel_bias"])[:, HEADS_PERM])
    j = np.arange(384)
    d = j - 127
    valid = (d >= 0) & (d <= 127)
    bk = t5_bucket_np(d)
    ohb = np.zeros((32, 384), np.float32)
    ohb[bk[valid], j[valid]] = 1.0
    sh["ohb"] = ohb
    sh["mask8"] = np.tile(np.where(valid, 0.0, -30000.0).astype(np.float32)[None, :], (8, 1))
    return sh


_PROG = {}


def kernel(**inputs):
    x = np.asarray(inputs["x"], dtype=np.float32)
    B, T, D = x.shape
    if T not in _PROG:
        _PROG[T] = build_program(T)
    nc = _PROG[T]
    sh = _host_layout(inputs)
    in_maps = []
    for b in range(B):
        m = dict(sh)
        m["x"] = np.ascontiguousarray(x[b])
        in_maps.append(m)
    res = run_bass_kernel_spmd(nc, in_maps, core_ids=list(range(B)))
    return np.stack([np.asarray(r["y"]) for r in res.results], axis=0).astype(np.float32)
```

```python
import numpy as np
from contextlib import ExitStack
import concourse.bass as bass
import concourse.mybir as mybir
from concourse.bass_utils import run_bass_kernel_spmd

F32 = mybir.dt.float32
BF16 = mybir.dt.bfloat16
I32 = mybir.dt.int32
U32 = mybir.dt.uint32
AF = mybir.ActivationFunctionType
ALU = mybir.AluOpType
AX = mybir.AxisListType

ENG_NAMES = ["pe", "act", "dve", "pool", "sp"]


class TT:
    def __init__(self, h, name, kind="sb"):
        self.h = h
        self.name = name
        self.kind = kind
        self.trk = self
        self.wev = {}
        self.rev = {}
        self.dsem = None
        self.dcnt = 0

    def __getitem__(self, idx):
        return self.h[idx]


class TTV(TT):
    def __init__(self, base, name, c0, n):
        TT.__init__(self, base.h, name, base.kind)
        self.trk = base.trk
        self.c0 = c0
        self.n = n

    def __getitem__(self, idx):
        if not isinstance(idx, tuple):
            idx = (idx, slice(None))
        r, c = idx
        start = 0 if c.start is None else c.start
        stop = self.n if c.stop is None else c.stop
        return self.h[r, self.c0 + start:self.c0 + stop]


def _freeze(fn):
    import types
    if fn.__closure__ is None:
        return fn
    cells = []
    for c in fn.__closure__:
        try:
            cells.append(types.CellType(c.cell_contents))
        except ValueError:
            cells.append(c)
    return types.FunctionType(fn.__code__, fn.__globals__, fn.__name__, fn.__defaults__, tuple(cells))


def _merge(d, src, skip=None):
    for k, (s, v) in src.items():
        if k == skip:
            continue
        if k not in d or d[k][1] < v:
            d[k] = (s, v)


class Sched:
    def __init__(self, nc, es):
        self.nc = nc
        self.es = es
        self.prog = {e: [] for e in ENG_NAMES}
        self.sem = {e: es.enter_context(nc.semaphore("s_" + e)) for e in ENG_NAMES}
        self.cnt = {e: 0 for e in ENG_NAMES}
        self.seen = {e: {} for e in ENG_NAMES}
        self.pend = {e: ([], []) for e in ENG_NAMES}
        self.final = {}
        self.ninst = 0
        self.nwait = 0
        self.ndsem = 0
        self.tts = []

    def _reg(self, t):
        self.tts.append(t)
        return t

    def sb(self, name, shape, dt, es=None):
        es = es or self.es
        return self._reg(TT(es.enter_context(self.nc.sbuf_tensor(name, list(shape), dt)), name))

    def ps(self, name, shape, dt=F32, es=None):
        es = es or self.es
        return self._reg(TT(es.enter_context(self.nc.psum_tensor(name, list(shape), dt)), name, "ps"))

    def dr(self, name, shape, dt, kind="Internal"):
        return self._reg(TT(self.nc.dram_tensor(name, list(shape), dt, kind=kind).ap(), name, "dr"))

    def alias(self, t, name):
        return self._reg(TT(t.h, name, t.kind))

    def _wait(self, eng, deps):
        for k, (s, v) in deps.items():
            if self.seen[eng].get(k, 0) < v:
                self.prog[eng].append(("w", s, v))
                self.seen[eng][k] = v
                self.nwait += 1

    def op(self, eng, fn, r=(), w=(), inc=True):
        w = [t.trk for t in w] + [t.trk for t in r if t.kind == "ps"]
        r = [t.trk for t in r if t.kind != "ps"]
        deps = {}
        for t in r:
            _merge(deps, t.wev)
        for t in w:
            _merge(deps, t.wev)
            _merge(deps, t.rev)
        if eng == "pe":
            deps.pop("s_pe", None)
        self._wait(eng, deps)
        self.ninst += 1
        pr, pw = self.pend[eng]
        pr.extend(r)
        pw.extend(w)
        self.prog[eng].append(("o", _freeze(fn), inc))
        if inc:
            self.cnt[eng] += 1
            k = "s_" + eng
            ev = (self.sem[eng], self.cnt[eng])
            for t in pw:
                t.wev = {k: ev}
                t.rev = {}
            for t in pr:
                if t not in pw:
                    t.rev[k] = ev
            self.pend[eng] = ([], [])

    def dma(self, q, dst, dst_ap, src, src_ap, final=False, nowaw=False, **kw):
        if dst is not None and dst.kind == "sb":
            owner = dst
        elif src is not None and src.kind == "sb":
            owner = src
        else:
            owner = dst if dst is not None else src
        if owner.dsem is None:
            owner.dsem = self.es.enter_context(self.nc.semaphore("d_" + owner.name))
            self.ndsem += 1
        k = "d_" + owner.name
        deps = {}
        if src is not None:
            _merge(deps, src.wev)
        if dst is not None:
            if not nowaw:
                _merge(deps, dst.wev, skip=k)
            _merge(deps, dst.rev)
        self._wait(q, deps)
        owner.dcnt += 16
        ev = (owner.dsem, owner.dcnt)
        self.prog[q].append(("d", lambda e: e.dma_start(out=dst_ap, in_=src_ap, **kw), owner.dsem))
        self.ninst += 1
        if dst is not None:
            dst.wev[k] = ev
            dst.rev = {}
        if src is not None:
            src.rev[k] = ev
        if final:
            self.final[k] = ev

    def emit(self):
        nc = self.nc
        for e in ENG_NAMES:
            assert not self.pend[e][0] and not self.pend[e][1], "unresolved pending on " + e
        for k, (s, v) in self.final.items():
            self.prog["sp"].append(("w", s, v))

        def replay(name, e):
            sem = self.sem[name]
            for it in self.prog[name]:
                if it[0] == "w":
                    e.wait_ge(it[1], it[2])
                elif it[0] == "o":
                    ins = it[1](e)
                    if it[2]:
                        ins.then_inc(sem, 1)
                else:
                    it[1](e).then_inc(it[2], 16)

        with nc.Block() as block:
            @block.tensor
            def _(e):
                replay("pe", e)

            @block.scalar
            def _(e):
                replay("act", e)

            @block.vector
            def _(e):
                replay("dve", e)

            @block.gpsimd
            def _(e):
                replay("pool", e)

            @block.sync
            def _(e):
                replay("sp", e)


def make_ident(S, ident, tmp):
    S.op("pool", lambda e: e.iota(out=tmp[:], pattern=[[1, 128]], base=0, channel_multiplier=-1), w=[tmp])
    S.op("dve", lambda e: e.tensor_single_scalar(out=ident[:], in_=tmp[:], scalar=0.0, op=ALU.is_equal), r=[tmp], w=[ident])


def cap(tt, offset, dims):
    t = tt[:]
    return bass.AP(t.tensor, offset, [[t.ap[0][0], t.ap[0][1]]] + [list(d) for d in dims])


EPS = 1e-6
HEADS_PERM = [0, 4, 1, 5, 2, 6, 3, 7]
NGRP = 64
CPG = 2
NSET = 4
R_RING = 4


def t5_bucket_np(d):
    d = np.maximum(d, 0)
    df = np.maximum(d, 1).astype(np.float32)
    large = 16 + (np.log(df / 16) / np.log(128 / 16) * 16).astype(np.int32)
    large = np.minimum(large, 31)
    return np.where(d < 16, d, large)


class Barrier:
    pass


def sched_barrier(S):
    tts = S.tts
    deps = {}
    for e in ENG_NAMES:
        if S.cnt[e] > 0:
            deps["s_" + e] = (S.sem[e], S.cnt[e])
    for t in tts:
        if t.dsem is not None and t.dcnt > 0:
            deps["d_" + t.name] = (t.dsem, t.dcnt)
    for e in ENG_NAMES:
        assert not S.pend[e][0] and not S.pend[e][1]
        S._wait(e, dict(deps))


def build_program(SEQ_T=4096, dbg=False):
    import os
    KSTOP = int(os.environ.get("KSTOP", "99"))
    KSUB = int(os.environ.get("KSUB", "99"))
    nc = bass.Bass("TRN2", target_bir_lowering=False)
    NB = SEQ_T // 128
    NT = SEQ_T // 256
    DI = lambda n, shp, dt=F32: nc.dram_tensor(n, list(shp), dt, kind="ExternalInput").ap()
    x_d = DI("x", [SEQ_T, 1024])
    win_d = DI("w_in", [1024, 4352])
    wco_d = DI("w_co", [512, 1024])
    wao_d = DI("w_ao", [512, 1024])
    wout_d = DI("w_out", [1024, 1024])
    wq_d = DI("w_q", [1024, 2048])
    skT_d = DI("skT", [128, 2048])
    utg_d = DI("utg", [NGRP, 128, 2048])
    vg_d = DI("vg", [NGRP, 128, 2048])
    gmix_d = DI("gmix", [128, 1024])
    gffn_d = DI("gffn", [128, 1024])
    bgate_d = DI("bgate", [128, 16])
    wconv_d = DI("wconv", [128, 12])
    qkn_d = DI("qkn", [128, 2])
    sink_d = DI("sinkr", [128, 8])
    relb_d = DI("relb", [32, 8])
    ohb_d = DI("ohb", [32, 384])
    mask8_d = DI("mask8", [8, 384])
    y_d = nc.dram_tensor("y", [SEQ_T, 1024], F32, kind="ExternalOutput").ap()
    dbg = bool(int(os.environ.get("KDBG", "0")))
    dbg_d = {}
    if dbg:
        for n_, w_ in (("sc", 2048), ("st1", 256), ("it1f", 256), ("ts", 128), ("pif", 128), ("pjf", 128), ("idx1f", 128), ("idx2f", 128), ("gat", 128),
                       ("idx1T", 256), ("gatT", 256), ("G0", 256), ("G5", 256), ("h2T0", 256), ("ge", 256)):
            dbg_d[n_] = nc.dram_tensor("dbg_" + n_, [128, w_], F32, kind="ExternalOutput").ap()
        for n_, w_ in (("h2Tb", 2048), ("qTsb", 4096), ("skTb", 2048), ("wqb", 2048)):
            dbg_d[n_] = nc.dram_tensor("dbg_" + n_, [128, w_], BF16, kind="ExternalOutput").ap()

    with ExitStack() as es:
        S = Sched(nc, es)
        x1_s = S.dr("x1_s", [SEQ_T, 1024], F32)
        es_s = S.dr("es_s", [NGRP, 128, 4096], BF16)
        r8_s = S.dr("r8_s", [8, 128, 384], F32)

        ident_bf = S.sb("ident_bf", [128, 128], BF16)
        ident_f = S.sb("ident_f", [128, 128], F32)
        iota_f = S.sb("iota_f", [128, 128], F32)
        gffn = S.sb("gffn_sb", [128, 1024], F32)
        eps_t = S.sb("eps_t", [128, 1], F32)
        S.op("dve", lambda e: e.memset(eps_t[:], EPS), w=[eps_t])

        with ExitStack() as e1:
            NSTG = 2
            cst_f = [S.sb("cst_f%d" % i, [128, 1024], F32, es=e1) for i in range(NSTG)]
            cst_b = [S.sb("cst_b%d" % i, [128, 1024], BF16, es=e1) for i in range(NSTG)]
            tmpi = S.sb("tmpi", [128, 128], I32, es=e1)
            make_ident(S, ident_bf, tmpi)
            S.op("dve", lambda e: e.tensor_single_scalar(out=ident_f[:], in_=tmpi[:], scalar=0.0, op=ALU.is_equal), r=[tmpi], w=[ident_f])
            tmpj = S.sb("tmpj", [128, 128], I32, es=e1)
            S.op("pool", lambda e: e.iota(out=tmpj[:], pattern=[[1, 128]], base=0, channel_multiplier=0), w=[tmpj])
            S.op("dve", lambda e: e.tensor_copy(out=iota_f[:], in_=tmpj[:]), r=[tmpj], w=[iota_f])
            blockones = S.sb("blockones", [128, 128], BF16, es=e1)
            S.op("dve", lambda e: e.memset(blockones[:], 0.0), w=[blockones])
            S.op("dve", lambda e: e.memset(blockones[0:64, 0:64], 1.0), w=[blockones])
            S.op("dve", lambda e: e.memset(blockones[64:128, 64:128], 1.0), w=[blockones])

            gmix = S.sb("gmix_sb", [128, 1024], F32, es=e1)
            bgate = S.sb("bgate_sb", [128, 16], F32, es=e1)
            wconv = S.sb("wconv_sb", [128, 12], F32, es=e1)
            qkn = S.sb("qkn_sb", [128, 2], F32, es=e1)
            gq = S.sb("gq_sb", [128, 1], F32, es=e1)
            negh1 = S.sb("negh1", [128, 1], F32, es=e1)
            S.op("dve", lambda e: e.memset(negh1[:], -0.5), w=[negh1])
            nbgate = S.sb("nbgate_sb", [128, 16], F32, es=e1)
            sinkr = S.sb("sinkr_sb", [128, 8], F32, es=e1)
            esink = S.sb("esink_sb", [128, 8], F32, es=e1)
            relb = S.sb("relb_sb", [32, 8], F32, es=e1)
            ohb = S.sb("ohb_sb", [32, 384], F32, es=e1)
            mask8 = S.sb("mask8_sb", [8, 384], F32, es=e1)
            r8 = S.sb("r8_sb", [8, 384], F32, es=e1)
            biasT = S.sb("biasT", [128, 2, 8, 128], F32, es=e1)
            for t, d in ((gmix, gmix_d), (gffn, gffn_d), (bgate, bgate_d), (wconv, wconv_d), (qkn, qkn_d),
                         (sinkr, sink_d), (relb, relb_d), (ohb, ohb_d), (mask8, mask8_d)):
                S.dma("sp", t, t[:], None, d)
            S.op("act", lambda e: e.mul(out=gq[:], in_=qkn[:, 0:1], mul=0.125), r=[qkn], w=[gq])
            S.op("act", lambda e: e.mul(out=nbgate[:], in_=bgate[:], mul=-1.0), r=[bgate], w=[nbgate])
            S.op("act", lambda e: e.activation(out=esink[:], in_=sinkr[:], func=AF.Exp), r=[sinkr], w=[esink])

            ps_l = S.ps("ps_l", [128, 1024], F32, es=e1)
            ps_v = [S.ps("ps_v%d" % i, [128, 512], F32, es=e1) for i in range(2)]
            ps_x = S.ps("ps_x", [128, 512], F32, es=e1)
            ps_tr = S.ps("ps_tr", [128, 8, 128], BF16, es=e1)
            ps_pb = [S.ps("ps_pb%d" % i, [128, 512], F32, es=e1) for i in range(2)]
            ps_p = [S._reg(TTV(ps_pb[0], "ps_p0", 0, 256)), S._reg(TTV(ps_pb[1], "ps_p1", 0, 256)), S._reg(TTV(ps_x, "ps_p2", 0, 256)),
                    S._reg(TTV(ps_v[0], "ps_p3", 0, 256)), S._reg(TTV(ps_v[1], "ps_p4", 0, 256)),
                    S._reg(TTV(ps_pb[0], "ps_p5", 256, 256)), S._reg(TTV(ps_pb[1], "ps_p6", 256, 256)), S._reg(TTV(ps_x, "ps_p7", 256, 256))]
            pp_i = [0]

            def next_pp():
                t = ps_p[pp_i[0] % len(ps_p)]
                pp_i[0] += 1
                return t

            S.op("pe", lambda e: e.matmul(ps_x[0:8, 0:384], lhsT=relb[:], rhs=ohb[:], start=True, stop=True), r=[relb, ohb], w=[ps_x])
            S.op("dve", lambda e: e.tensor_tensor(out=r8[:], in0=ps_x[0:8, 0:384], in1=mask8[:], op=ALU.add), r=[ps_x, mask8], w=[r8])
            r8a = r8[:]
            S.dma("sp", r8_s, r8_s[:], r8, bass.AP(r8a.tensor, 0, [[r8a.ap[0][0], 8], [0, 128], [1, 384]]))
            r8t = r8_s[:].tensor
            for part, off in ((0, 255), (1, 127)):
                src = bass.AP(r8t, off, [[383, 128], [128 * 384, 8], [1, 128]])
                S.dma("sp", biasT, biasT[:, part, :, :], r8_s, src)

            w_in = S.sb("w_in_bf", [128, 8, 4352], BF16, es=e1)
            wco = S.sb("wco_bf", [128, 4, 1024], BF16, es=e1)
            wao = S.sb("wao_bf", [128, 4, 1024], BF16, es=e1)
            wout = S.sb("wout_bf", [128, 8, 1024], BF16, es=e1)
            with ExitStack() as e0:
                stg = [S.sb("stg%d" % i, [128, 2176], F32, es=e0) for i in range(6)]
                pieces = []
                for kc in range(8):
                    for hf in range(2):
                        pieces.append((win_d[kc * 128:(kc + 1) * 128, hf * 2176:(hf + 1) * 2176], w_in, w_in[:, kc, hf * 2176:(hf + 1) * 2176], 2176))
                for cc in range(4):
                    pieces.append((wco_d[cc * 128:(cc + 1) * 128, :], wco, wco[:, cc, :], 1024))
                    pieces.append((wao_d[cc * 128:(cc + 1) * 128, :], wao, wao[:, cc, :], 1024))
                for kc in range(8):
                    pieces.append((wout_d[kc * 128:(kc + 1) * 128, :], wout, wout[:, kc, :], 1024))
                for i, (src, dt_, dap, n) in enumerate(pieces):
                    S.dma("pool", dt_, dap, None, src)
                sched_barrier(S)

            cast_jobs = []
            for g in range(NGRP):
                for q in range(2):
                    cast_jobs.append((utg_d[g, :, q * 1024:(q + 1) * 1024], es_s, es_s[g, :, q * 1024:(q + 1) * 1024]))
                    cast_jobs.append((vg_d[g, :, q * 1024:(q + 1) * 1024], es_s, es_s[g, :, 2048 + q * 1024:2048 + (q + 1) * 1024]))
            cast_state = {"ld": 0, "cv": 0}

            def cast_loads():
                pass

            def cast_step(n_conv):
                for _ in range(n_conv):
                    i = cast_state["cv"]
                    if i >= len(cast_jobs):
                        return
                    S.dma("pool", cast_jobs[i][1], cast_jobs[i][2], None, cast_jobs[i][0], nowaw=True)
                    cast_state["cv"] += 1

            JOBS_PER_ST = (len(cast_jobs) + NT - 1) // NT
            tick_state = {"n": 0}

            def cast_tick():
                if KSTOP < 3:
                    return
                tick_state["n"] += 1
                if tick_state["n"] <= JOBS_PER_ST:
                    cast_step(1)

            xt = [S.sb("xt%d" % i, [128, 2, 1024], F32, es=e1) for i in range(2)]
            x1t = [S.sb("x1t%d" % i, [128, 2, 1024], F32, es=e1) for i in range(1)]
            ss = S.sb("ss", [128, 4], F32, es=e1)
            sd = S.sb("sd", [128, 4], F32, es=e1)
            rstd = S.sb("rstd", [128, 4], F32, es=e1)
            xn = [S.sb("xn%d" % i, [128, 1024], BF16, es=e1) for i in range(2)]
            hTs = [S.sb("hT%d" % i, [128, 8, 256], BF16, es=e1) for i in range(2)]
            u_sb = S.sb("u_sb", [128, 256], F32, es=e1)
            hcv = [S.sb("hcv%d" % i, [128, 258], F32, es=e1) for i in range(4)]
            cacc = S.sb("cacc", [128, 256], F32, es=e1)
            ycTs = [S.sb("ycT%d" % i, [128, 4, 256], BF16, es=e1) for i in range(2)]
            sq = S.sb("sq", [128, 256], BF16, es=e1)
            sdq = S.sb("sdq", [128, 256], F32, es=e1)
            qT = S.sb("qT", [128, 4, 256], BF16, es=e1)
            kA = [S.sb("kA%d" % i, [128, 128], BF16, es=e1) for i in range(R_RING)]
            kB = [S.sb("kB%d" % i, [128, 128], BF16, es=e1) for i in range(R_RING)]
            vaug = [S.sb("vaug%d" % i, [128, 2, 65], BF16, es=e1) for i in range(R_RING)]
            lb = S.sb("lb", [128, 1024], F32, es=e1)
            pT = [S.sb("pT%d" % i, [128, 1024], BF16, es=e1) for i in range(2)]
            dn = S.sb("dn", [128, 8], F32, es=e1)
            rd = S.sb("rd", [128, 8], F32, es=e1)
            atok = S.sb("atok", [128, 512], BF16, es=e1)
            attnTs = [S.sb("attnT%d" % i, [128, 4, 256], BF16, es=e1) for i in range(2)]
            sgc = S.sb("sgc", [128, 256], F32, es=e1)
            sga = S.sb("sga", [128, 256], F32, es=e1)
            mixT = S.sb("mixT", [128, 8, 256], BF16, es=e1)

            for i in range(R_RING):
                S.op("dve", lambda e, i=i: e.memset(kA[i][:], 0.0), w=[kA[i]])
                S.op("dve", lambda e, i=i: e.memset(kB[i][:], 0.0), w=[kB[i]])
                S.op("dve", lambda e, i=i: e.memset(vaug[i][:], 1.0), w=[vaug[i]])
            for i in range(4):
                S.op("dve", lambda e, i=i: e.memset(hcv[i][:], 0.0), w=[hcv[i]])

            def proj(out_t, col0, hT):
                for kc in range(8):
                    S.op("pe", lambda e, kc=kc: e.matmul(out_t[:, 0:256], lhsT=w_in[:, kc, col0:col0 + 128], rhs=hT[:, kc, :],
                                                         start=(kc == 0), stop=(kc == 7)),
                         r=[w_in, hT], w=[out_t], inc=(kc == 7))

            def norm_T(xsrc, bi, gam, xn_t, dstT, ptr):
                S.op("act", lambda e: e.activation(out=xn_t[:], in_=xsrc[:, bi, :], func=AF.Square, accum_out=ss[:, bi:bi + 1]),
                     r=[xsrc], w=[xn_t, ss])
                S.op("dve", lambda e: e.tensor_scalar(out=sd[:, bi:bi + 1], in0=ss[:, bi:bi + 1], scalar1=1.0 / 1024, scalar2=EPS, op0=ALU.mult, op1=ALU.add), r=[ss], w=[sd])
                S.op("pool", lambda e: e.tensor_tensor(out=rstd[:, bi:bi + 1], in0=sd[:, bi:bi + 1], in1=negh1[:, 0:1], op=ALU.pow), r=[sd, negh1], w=[rstd])
                S.op("dve", lambda e: e.scalar_tensor_tensor(out=xn_t[:], in0=xsrc[:, bi, :], scalar=rstd[:, bi:bi + 1], in1=gam[:],
                                                             op0=ALU.mult, op1=ALU.mult), r=[xsrc, rstd, gam], w=[xn_t])
                for kc in range(8):
                    S.op("pe", lambda e, kc=kc: e.transpose(out=ptr[:, kc, :], in_=xn_t[:, kc * 128:(kc + 1) * 128], identity=ident_bf[:]),
                         r=[xn_t, ident_bf], w=[ptr], inc=(kc == 7))
                S.op("act", lambda e: e.copy(out=dstT[:, :, bi * 128:(bi + 1) * 128], in_=ptr[:]), r=[ptr], w=[dstT])

            EPS_AP = [eps_t]

            S.dma("sp", xt[0], xt[0][:], None, x_d[0:256, :].rearrange("(b p) d -> p b d", p=128))
            def stageA(st):
                xs = xt[st % 2]
                hTa, ycTa, attnTa = hTs[st % 2], ycTs[st % 2], attnTs[st % 2]
                for bi in range(2):
                    norm_T(xs, bi, gmix, xn[bi], hTa, ps_tr)
                    yield
                def conv_it(cc):
                    pu, pc, pb = next_pp(), next_pp(), next_pp()
                    proj(pu, cc * 128, hTa)
                    proj(pc, 1024 + cc * 128, hTa)
                    proj(pb, 512 + cc * 128, hTa)
                    hc = hcv[cc]
                    S.op("act", lambda e, pu=pu: e.copy(out=u_sb[:], in_=pu[:]), r=[pu], w=[u_sb])
                    S.op("dve", lambda e, pc=pc, hc=hc: e.tensor_tensor(out=hc[:, 2:258], in0=pc[:], in1=u_sb[:], op=ALU.mult), r=[pc, u_sb], w=[hc])
                    S.op("dve", lambda e, hc=hc, cc=cc: e.tensor_scalar(out=cacc[:], in0=hc[:, 2:258], scalar1=wconv[:, 8 + cc:9 + cc], scalar2=None, op0=ALU.mult),
                         r=[hc, wconv], w=[cacc])
                    S.op("dve", lambda e, hc=hc, cc=cc: e.scalar_tensor_tensor(out=cacc[:], in0=hc[:, 1:257], scalar=wconv[:, 4 + cc:5 + cc], in1=cacc[:],
                                                                             op0=ALU.mult, op1=ALU.add), r=[hc, wconv, cacc], w=[cacc])
                    S.op("dve", lambda e, hc=hc, cc=cc: e.scalar_tensor_tensor(out=cacc[:], in0=hc[:, 0:256], scalar=wconv[:, cc:cc + 1], in1=cacc[:],
                                                                             op0=ALU.mult, op1=ALU.add), r=[hc, wconv, cacc], w=[cacc])
                    S.op("dve", lambda e, pb=pb, cc=cc: e.tensor_tensor(out=ycTa[:, cc, :], in0=pb[:], in1=cacc[:], op=ALU.mult), r=[pb, cacc], w=[ycTa])
                    S.op("act", lambda e, hc=hc: e.copy(out=hc[:, 0:2], in_=hc[:, 256:258]), r=[hc], w=[hc])

                def qk_it(j):
                    pq = next_pp()
                    proj(pq, 1536 + j * 128, hTa)
                    S.op("act", lambda e, pq=pq: e.activation(out=sq[:], in_=pq[:], func=AF.Square), r=[pq], w=[sq])
                    pss = next_pp()
                    S.op("pe", lambda e, pss=pss: e.matmul(pss[:], lhsT=blockones[:], rhs=sq[:], start=True, stop=True), r=[blockones, sq], w=[pss])
                    S.op("act", lambda e, pss=pss: e.activation(out=sdq[:], in_=pss[:], func=AF.Ln, scale=1.0 / 64, bias=eps_t[:, 0:1]), r=[pss, eps_t], w=[sdq])
                    S.op("act", lambda e: e.activation(out=sdq[:], in_=sdq[:], func=AF.Exp, scale=-0.5), r=[sdq], w=[sdq])
                    if j < 4:
                        S.op("dve", lambda e, pq=pq, j=j: e.scalar_tensor_tensor(out=qT[:, j, :], in0=pq[:], scalar=gq[:, 0:1], in1=sdq[:],
                                                                               op0=ALU.mult, op1=ALU.mult), r=[pq, gq, sdq], w=[qT])
                    else:
                        for bi in range(2):
                            slot = (2 * st + bi) % R_RING
                            S.op("dve", lambda e, pq=pq, bi=bi, slot=slot: e.scalar_tensor_tensor(
                                out=kA[slot][0:64, :], in0=pq[0:64, bi * 128:(bi + 1) * 128], scalar=qkn[0:64, 1:2], in1=sdq[0:64, bi * 128:(bi + 1) * 128],
                                op0=ALU.mult, op1=ALU.mult), r=[pq, qkn, sdq], w=[kA[slot]])
                            S.op("dve", lambda e, pq=pq, bi=bi, slot=slot: e.scalar_tensor_tensor(
                                out=kB[slot][64:128, :], in0=pq[64:128, bi * 128:(bi + 1) * 128], scalar=qkn[64:128, 1:2], in1=sdq[64:128, bi * 128:(bi + 1) * 128],
                                op0=ALU.mult, op1=ALU.mult), r=[pq, qkn, sdq], w=[kB[slot]])


                for i5 in range(5):
                    qk_it(i5)
                    cast_tick()
                    yield
                    if i5 < 4:
                        conv_it(i5)
                        cast_tick()
                        yield
                for bi in range(2 if KSUB >= 4 else 0):
                    slot = (2 * st + bi) % R_RING
                    pv = next_pp()
                    for kc in range(8):
                        S.op("pe", lambda e, kc=kc, bi=bi, pv=pv: e.matmul(pv[:, 0:128], lhsT=hTa[:, kc, bi * 128:(bi + 1) * 128], rhs=w_in[:, kc, 2176:2304],
                                                                           start=(kc == 0), stop=(kc == 7)), r=[hTa, w_in], w=[pv], inc=(kc == 7))
                    S.op("act", lambda e, pv=pv, slot=slot: e.copy(out=vaug[slot][:, :, 0:64], in_=pv[:, 0:128].rearrange("p (k d) -> p k d", k=2)),
                         r=[pv], w=[vaug[slot]])
                for bi in range(2 if KSUB >= 5 else 0):
                    gb = 2 * st + bi
                    parts = [1] if gb == 0 else [0, 1]
                    for part in parts:
                        kslot = (gb - 1 + part) % R_RING
                        for hs in range(8):
                            kt = kA[kslot] if hs % 2 == 0 else kB[kslot]
                            S.op("pe", lambda e, kt=kt, hs=hs, bi=bi: e.matmul(ps_l[:, hs * 128:(hs + 1) * 128], lhsT=kt[:], rhs=qT[:, hs // 2, bi * 128:(bi + 1) * 128],
                                                                              start=True, stop=True), r=[kt, qT], w=[ps_l], inc=(hs == 7))
                        S.op("dve", lambda e, part=part: e.tensor_tensor(out=lb[:], in0=ps_l[:], in1=biasT[:, part, :, :].rearrange("p h t -> p (h t)"), op=ALU.add),
                             r=[ps_l, biasT], w=[lb])
                        S.op("act", lambda e, part=part: e.activation(out=pT[part][:], in_=lb[:], func=AF.Exp), r=[lb], w=[pT[part]])
                    for hs in range(8):
                        pvt = ps_v[hs // 4]
                        for pi, part in enumerate(parts):
                            kslot = (gb - 1 + part) % R_RING
                            S.op("pe", lambda e, pvt=pvt, hs=hs, part=part, kslot=kslot, pi=pi: e.matmul(
                                pvt[:, (hs % 4) * 128:(hs % 4) * 128 + 65], lhsT=pT[part][:, hs * 128:(hs + 1) * 128], rhs=vaug[kslot][:, hs % 2, :],
                                start=(pi == 0), stop=(pi == len(parts) - 1)), r=[pT[part], vaug[kslot]], w=[pvt], inc=(pi == len(parts) - 1 and hs % 4 == 3))
                    for g2 in range(2):
                        pv3 = ps_v[g2][:].rearrange("p (h c) -> p h c", h=4)
                        S.op("dve", lambda e, g2=g2, pv3=pv3: e.tensor_tensor(out=dn[:, g2 * 4:(g2 + 1) * 4], in0=pv3[:, :, 64], in1=esink[:, g2 * 4:(g2 + 1) * 4], op=ALU.add),
                             r=[ps_v[g2], esink], w=[dn])
                    S.op("dve", lambda e: e.reciprocal(out=rd[:], in_=dn[:]), r=[dn], w=[rd])
                    for g2 in range(2):
                        pv3 = ps_v[g2][:].rearrange("p (h c) -> p h c", h=4)
                        S.op("dve", lambda e, g2=g2, pv3=pv3: e.tensor_tensor(
                            out=atok[:, g2 * 256:(g2 + 1) * 256].rearrange("p (h d) -> p h d", h=4), in0=pv3[:, :, 0:64],
                            in1=rd[:, g2 * 4:(g2 + 1) * 4].unsqueeze(2).to_broadcast([128, 4, 64]), op=ALU.mult), r=[ps_v[g2], rd], w=[atok])
                    for j in range(4):
                        S.op("pe", lambda e, j=j: e.transpose(out=ps_tr[:, j, :], in_=atok[:, j * 128:(j + 1) * 128], identity=ident_bf[:]),
                             r=[atok, ident_bf], w=[ps_tr], inc=(j == 3))
                    S.op("act", lambda e, bi=bi: e.copy(out=attnTa[:, :, bi * 128:(bi + 1) * 128], in_=ps_tr[:, 0:4, :]), r=[ps_tr], w=[attnTa])
                    cast_tick()
                    yield

            def stageB(st):
                xs = xt[st % 2]
                hTb, ycTb, attnTb = hTs[st % 2], ycTs[st % 2], attnTs[st % 2]
                for dc in range(8 if KSUB >= 6 else 0):
                    pyc, pya, pgc, pga = next_pp(), next_pp(), next_pp(), next_pp()
                    for cc in range(4):
                        S.op("pe", lambda e, cc=cc, dc=dc, pyc=pyc: e.matmul(pyc[:], lhsT=wco[:, cc, dc * 128:(dc + 1) * 128], rhs=ycTb[:, cc, :], start=(cc == 0), stop=(cc == 3)),
                             r=[wco, ycTb], w=[pyc], inc=(cc == 3))
                    for cc in range(4):
                        S.op("pe", lambda e, cc=cc, dc=dc, pya=pya: e.matmul(pya[:], lhsT=wao[:, cc, dc * 128:(dc + 1) * 128], rhs=attnTb[:, cc, :], start=(cc == 0), stop=(cc == 3)),
                             r=[wao, attnTb], w=[pya], inc=(cc == 3))
                    proj(pgc, 2304 + dc * 128, hTb)
                    proj(pga, 3328 + dc * 128, hTb)
                    for pg_, sg_, col_ in ((pgc, sgc, dc), (pga, sga, 8 + dc)):
                        S.op("act", lambda e: e.activation(out=sg_[:], in_=pg_[:], func=AF.Exp, scale=-1.0, bias=nbgate[:, col_:col_ + 1]), r=[pg_, nbgate], w=[sg_])
                        S.op("act", lambda e: e.activation(out=sg_[:], in_=sg_[:], func=AF.Ln, bias=1.0), r=[sg_], w=[sg_])
                        S.op("act", lambda e: e.activation(out=sg_[:], in_=sg_[:], func=AF.Exp, scale=-1.0), r=[sg_], w=[sg_])
                    S.op("dve", lambda e, pyc=pyc: e.tensor_tensor(out=sgc[:], in0=pyc[:], in1=sgc[:], op=ALU.mult), r=[pyc, sgc], w=[sgc])
                    S.op("dve", lambda e, pya=pya: e.tensor_tensor(out=sga[:], in0=pya[:], in1=sga[:], op=ALU.mult), r=[pya, sga], w=[sga])
                    S.op("dve", lambda e, dc=dc: e.tensor_tensor(out=mixT[:, dc, :], in0=sgc[:], in1=sga[:], op=ALU.add), r=[sgc, sga], w=[mixT])
                    cast_tick()
                    yield
                xo = x1t[0]
                for bi in range(2 if KSUB >= 7 else 0):
                    for hf in range(2):
                        for kc in range(8):
                            S.op("pe", lambda e, kc=kc, bi=bi, hf=hf: e.matmul(ps_x[:], lhsT=mixT[:, kc, bi * 128:(bi + 1) * 128], rhs=wout[:, kc, hf * 512:(hf + 1) * 512],
                                                                              start=(kc == 0), stop=(kc == 7)), r=[mixT, wout], w=[ps_x], inc=(kc == 7))
                        S.op("dve", lambda e, bi=bi, hf=hf, xo=xo, xs=xs: e.tensor_tensor(out=xo[:, bi, hf * 512:(hf + 1) * 512], in0=ps_x[:], in1=xs[:, bi, hf * 512:(hf + 1) * 512], op=ALU.add),
                             r=[ps_x, xs], w=[xo])
                        yield
                S.dma("sp", x1_s, x1_s[st * 256:(st + 1) * 256, :].rearrange("(b p) d -> p b d", p=128), xo, xo[:], nowaw=True)
                if KSTOP < 4:
                    S.dma("sp", None, y_d[st * 256:(st + 1) * 256, :].rearrange("(b p) d -> p b d", p=128), xo, xo[:], final=True)

            if KSTOP >= 2:
                if KSTOP >= 3:
                    cast_loads()
                if NT > 1:
                    S.dma("sp", xt[1], xt[1][:], None, x_d[256:512, :].rearrange("(b p) d -> p b d", p=128))
                for _ in stageA(0):
                    pass
                for st in range(NT):
                    tick_state["n"] = 0
                    gB = stageB(st)
                    gA = stageA(st + 1) if st + 1 < NT else iter(())
                    aliveA = aliveB = True
                    while aliveA or aliveB:
                        if aliveB:
                            aliveB = next(gB, "END") != "END"
                        if aliveA:
                            aliveA = next(gA, "END") != "END"
                    if st + 2 < NT:
                        S.dma("sp", xt[st % 2], xt[st % 2][:], None, x_d[(st + 2) * 256:(st + 3) * 256, :].rearrange("(b p) d -> p b d", p=128))
            if KSTOP >= 3:
                cast_step(len(cast_jobs))
            sched_barrier(S)

        if KSTOP >= 4:
            build_phase2(nc, S, es, locals())
        S.emit()
    return nc


def build_phase2(nc, S, es, L):
    NT = L["NT"]
    x1_s, es_s = L["x1_s"], L["es_s"]
    ident_f, iota_f, gffn, eps_t = L["ident_f"], L["iota_f"], L["gffn"], L["eps_t"]
    wq_d, skT_d, y_d = L["wq_d"], L["skT_d"], L["y_d"]
    dbg_d = L["dbg_d"]
    with ExitStack() as e2:
        wq = S.sb("wq_bf", [128, 8, 2048], BF16, es=e2)
        skT = S.sb("skT_bf", [128, 16, 128], BF16, es=e2)
        with ExitStack() as e0:
            stg = [S.sb("stgq%d" % i, [128, 2048], F32, es=e0) for i in range(4)]
            for kc in range(8):
                S.dma("pool", wq, wq[:, kc, :], None, wq_d[kc * 128:(kc + 1) * 128, :])
            S.dma("pool", skT, skT[:].rearrange("p a b -> p (a b)"), None, skT_d)
            sched_barrier(S)

        acc = [S.ps("acc%d" % i, [128, 512], F32, es=e2) for i in range(4)]
        a_pb = [S.ps("a_pb%d" % i, [128, 512], F32, es=e2) for i in range(2)]
        a_ps = [S._reg(TTV(a_pb[i], "a_ps%d" % i, 0, 256)) for i in range(2)]
        pm = [S.ps("pm%d" % i, [128, 512], F32, es=e2) for i in range(2)]
        pm_i = [0]

        def next_pm():
            t = pm[pm_i[0] % 2]
            pm_i[0] += 1
            return t

        x1t = [S.sb("x1b%d" % i, [128, 2, 1024], F32, es=e2) for i in range(2)]
        ss = S.sb("ss2", [128, 2], F32, es=e2)
        sd = S.sb("sd2", [128, 2], F32, es=e2)
        rstd = S.sb("rstd2", [128, 2], F32, es=e2)
        negh = S.sb("negh", [128, 2], F32, es=e2)
        S.op("dve", lambda e: e.memset(negh[:], -0.5), w=[negh])
        xn = S.sb("xn2", [128, 1024], F32, es=e2)
        h2T = [S.sb("h2T%d" % i, [128, 8, 256], BF16, es=e2) for i in range(2)]
        qTs = S.sb("qTs", [128, 16, 256], BF16, es=e2)
        bigB = S.sb("bigB", [128, 2048], F32, es=e2)
        st1 = S.sb("st1", [128, 16, 16], F32, es=e2)
        it1 = S.sb("it1", [128, 16, 16], U32, es=e2)
        st1_a = [S.alias(st1, "st1_%d" % i) for i in range(16)]
        it1_a = [S.alias(it1, "it1_%d" % i) for i in range(16)]
        it1f = S.sb("it1f", [128, 16, 16], F32, es=e2)
        wk = [S.sb("wk%d" % i, [128, 128], F32, es=e2) for i in range(2)]
        cw = [S.sb("cw%d" % i, [128, 256], F32, es=e2) for i in range(2)]
        ts = S.sb("ts", [128, 8, 16], F32, es=e2)
        pos = S.sb("pos", [128, 8, 16], U32, es=e2)
        ts_a = [S.alias(ts, "ts_%d" % i) for i in range(8)]
        pos_a = [S.alias(pos, "pos_%d" % i) for i in range(8)]
        posi = S.sb("posi", [128, 8, 16], U32, es=e2)
        posj = S.sb("posj", [128, 8, 16], U32, es=e2)
        pif = S.sb("pif", [128, 8, 16], F32, es=e2)
        pjf = S.sb("pjf", [128, 8, 16], F32, es=e2)
        negm = S.sb("negm", [128, 8], F32, es=e2)
        eg = S.sb("eg", [128, 8, 16], F32, es=e2)
        sume = S.sb("sume", [128, 8], F32, es=e2)
        rse = S.sb("rse", [128, 8], F32, es=e2)
        gat = S.sb("gat", [128, 128], F32, es=e2)
        idx1f = S.sb("idx1f", [128, 128], F32, es=e2)
        idx2f = S.sb("idx2f", [128, 128], F32, es=e2)
        idx1T = S.sb("idx1T", [128, 256], F32, es=e2)
        idx2T = S.sb("idx2T", [128, 256], F32, es=e2)
        gatT = S.sb("gatT", [128, 256], F32, es=e2)
        nidx2T = S.sb("nidx2T", [128, 256], F32, es=e2)
        gatTs = S.sb("gatTs", [128, 256], F32, es=e2)
        NOH = 12
        Rt = [S.sb("Rt%d" % i, [128, 128], BF16, es=e2) for i in range(NOH)]
        Lt = [S.sb("Lt%d" % i, [128, 128], BF16, es=e2) for i in range(NOH)]
        G_sb = S.sb("G_sb", [128, 128, 256], BF16, es=e2)
        EX = [S.sb("EX%d" % i, [128, 4096], BF16, es=e2) for i in range(NSET)]
        ge = [S.sb("ge%d" % i, [128, 256], BF16, es=e2) for i in range(2)]
        GA = [S.sb("GA%d" % i, [128, 256], BF16, es=e2) for i in range(3)]
        iota_b = S.sb("iota_b", [128, 128], BF16, es=e2)
        S.op("dve", lambda e: e.tensor_copy(out=iota_b[:], in_=iota_f[:]), r=[iota_f], w=[iota_b])
        gring = [pm[0], pm[1]] + acc
        g_i = [0]

        def next_g():
            t = gring[g_i[0] % len(gring)]
            g_i[0] += 1
            return t

        def front(ti):
            xs = x1t[ti % 2]
            hT = h2T[ti % 2]
            S.dma("sp", xs, xs[:], x1_s, x1_s[ti * 256:(ti + 1) * 256, :].rearrange("(b p) d -> p b d", p=128))
            for bi in range(2):
                S.op("dve", lambda e, bi=bi: e.scalar_tensor_tensor(out=xn[:], in0=xs[:, bi, :], scalar=1.0, in1=xs[:, bi, :], op0=ALU.mult, op1=ALU.mult, accum_out=ss[:, bi:bi + 1]),
                     r=[xs], w=[xn, ss])
            S.op("dve", lambda e: e.tensor_scalar(out=sd[:], in0=ss[:], scalar1=1.0 / 1024, scalar2=EPS, op0=ALU.mult, op1=ALU.add), r=[ss], w=[sd])
            yield
            S.op("pool", lambda e: e.tensor_tensor(out=rstd[:], in0=sd[:], in1=negh[:], op=ALU.pow), r=[sd, negh], w=[rstd])
            yield
            for bi in range(2):
                S.op("dve", lambda e, bi=bi: e.scalar_tensor_tensor(out=xn[:], in0=xs[:, bi, :], scalar=rstd[:, bi:bi + 1], in1=gffn[:], op0=ALU.mult, op1=ALU.mult),
                     r=[xs, rstd, gffn], w=[xn])
                yield
                for rnd in range(2):
                    p = next_pm()
                    for k4 in range(4):
                        kc = rnd * 4 + k4
                        S.op("pe", lambda e, p=p, k4=k4, kc=kc: e.transpose(out=p[:, k4 * 128:(k4 + 1) * 128], in_=xn[:, kc * 128:(kc + 1) * 128], identity=ident_f[:]),
                             r=[xn, ident_f], w=[p], inc=(k4 == 3))
                    S.op("act", lambda e, p=p, rnd=rnd, bi=bi: e.copy(out=hT[:, rnd * 4:(rnd + 1) * 4, bi * 128:(bi + 1) * 128], in_=p[:].rearrange("p (k t) -> p k t", k=4)),
                         r=[p], w=[hT])
                    yield
            for hp in range(16):
                p = next_pm()
                for kc in range(8):
                    S.op("pe", lambda e, p=p, kc=kc, hp=hp: e.matmul(p[:, 0:256], lhsT=wq[:, kc, hp * 128:(hp + 1) * 128], rhs=hT[:, kc, :], start=(kc == 0), stop=(kc == 7)),
                         r=[wq, hT], w=[p], inc=(kc == 7))
                S.op("act", lambda e, p=p, hp=hp: e.copy(out=qTs[:, hp, :], in_=p[:, 0:256]), r=[p], w=[qTs])
                yield
            for tsub in range(2):
                for g4 in range(4):
                    p = next_pm()
                    for k4 in range(4):
                        hp = g4 * 4 + k4
                        S.op("pe", lambda e, p=p, k4=k4, hp=hp: e.matmul(p[:, k4 * 128:(k4 + 1) * 128], lhsT=qTs[:, hp, tsub * 128:(tsub + 1) * 128], rhs=skT[:, hp, :], start=True, stop=True),
                             r=[qTs, skT], w=[p], inc=(k4 == 3))
                    S.op("act", lambda e, p=p, g4=g4: e.copy(out=bigB[:, g4 * 512:(g4 + 1) * 512], in_=p[:]), r=[p], w=[bigB])
                    yield
                for hp0 in range(0, 16, 2):
                    pr = (hp0, hp0 + 1)
                    for hp in pr:
                        S.op("dve", lambda e, hp=hp: e.max(out=st1[:, hp, 0:8], in_=bigB[:, hp * 128:(hp + 1) * 128]), r=[bigB], w=[st1_a[hp]])
                    for hp in pr:
                        S.op("dve", lambda e, hp=hp: e.max_index(out=it1[:, hp, 0:8], in_max=st1[:, hp, 0:8], in_values=bigB[:, hp * 128:(hp + 1) * 128]), r=[bigB, st1_a[hp]], w=[it1_a[hp]])
                    for hp in pr:
                        w_ = wk[hp % 2]
                        S.op("dve", lambda e, hp=hp, w_=w_: e.match_replace(out=w_[:], in_to_replace=st1[:, hp, 0:8], in_values=bigB[:, hp * 128:(hp + 1) * 128], imm_value=-1e30), r=[bigB, st1_a[hp]], w=[w_])
                    for hp in pr:
                        w_ = wk[hp % 2]
                        S.op("dve", lambda e, hp=hp, w_=w_: e.max(out=st1[:, hp, 8:16], in_=w_[:]), r=[w_], w=[st1_a[hp]])
                    for hp in pr:
                        w_ = wk[hp % 2]
                        S.op("dve", lambda e, hp=hp, w_=w_: e.max_index(out=it1[:, hp, 8:16], in_max=st1[:, hp, 8:16], in_values=w_[:]), r=[w_, st1_a[hp]], w=[it1_a[hp]])
                    yield
                for q4 in range(4):
                    cand4 = cap(bigB, q4 * 512, [[256, 2], [16, 16], [1, 16]])
                    in0 = cap(st1, q4 * 64, [[32, 2], [1, 16], [0, 16]])
                    in1 = cap(st1, q4 * 64 + 16, [[32, 2], [0, 16], [1, 16]])
                    S.op("dve", lambda e: e.tensor_tensor(out=cand4, in0=in0, in1=in1, op=ALU.add), r=st1_a, w=[bigB])
                    yield
                for h0 in range(0, 8, 2):
                    pr = (h0, h0 + 1)
                    cins = {h: bigB[:, h * 256:(h + 1) * 256] for h in pr}
                    for h in pr:
                        S.op("dve", lambda e, h=h, cin=cins[h]: e.max(out=ts[:, h, 0:8], in_=cin), r=[bigB], w=[ts_a[h]])
                    for h in pr:
                        S.op("dve", lambda e, h=h, cin=cins[h]: e.max_index(out=pos[:, h, 0:8], in_max=ts[:, h, 0:8], in_values=cin), r=[bigB, ts_a[h]], w=[pos_a[h]])
                    for h in pr:
                        c_ = cw[h % 2]
                        S.op("dve", lambda e, h=h, cin=cins[h], c_=c_: e.match_replace(out=c_[:], in_to_replace=ts[:, h, 0:8], in_values=cin, imm_value=-1e30), r=[bigB, ts_a[h]], w=[c_])
                    for h in pr:
                        c_ = cw[h % 2]
                        S.op("dve", lambda e, h=h, c_=c_: e.max(out=ts[:, h, 8:16], in_=c_[:]), r=[c_], w=[ts_a[h]])
                    for h in pr:
                        c_ = cw[h % 2]
                        S.op("dve", lambda e, h=h, c_=c_: e.max_index(out=pos[:, h, 8:16], in_max=ts[:, h, 8:16], in_values=c_[:]), r=[c_, ts_a[h]], w=[pos_a[h]])
                    yield
                S.op("dve", lambda e: e.tensor_tensor(out=eg[:], in0=ts[:], in1=ts[:, :, 0:1].to_broadcast([128, 8, 16]), op=ALU.subtract), r=ts_a, w=[eg])
                yield
                S.op("dve", lambda e: e.tensor_single_scalar(out=posi[:], in_=pos[:], scalar=4, op=ALU.logical_shift_right), r=pos_a, w=[posi])
                S.op("dve", lambda e: e.tensor_single_scalar(out=posj[:], in_=pos[:], scalar=15, op=ALU.bitwise_and), r=pos_a, w=[posj])
                S.op("dve", lambda e: e.tensor_copy(out=pif[:], in_=posi[:]), r=[posi], w=[pif])
                S.op("dve", lambda e: e.tensor_copy(out=pjf[:], in_=posj[:]), r=[posj], w=[pjf])
                S.op("dve", lambda e: e.tensor_copy(out=it1f[:], in_=it1[:]), r=it1_a, w=[it1f])
                yield
                io4 = cap(iota_f, 0, [[0, 2], [0, 16], [1, 16]])
                for which, pf, dst in ((0, pif, idx1f), (1, pjf, idx2f)):
                    for q4 in range(4):
                        eq4 = cap(bigB, q4 * 512, [[256, 2], [16, 16], [1, 16]])
                        sel = cap(pf, q4 * 32, [[16, 2], [1, 16], [0, 16]])
                        itb = cap(it1f, q4 * 64 + which * 16, [[32, 2], [0, 16], [1, 16]])
                        S.op("dve", lambda e: e.tensor_tensor(out=eq4, in0=io4, in1=sel, op=ALU.is_equal), r=[iota_f, pf], w=[bigB])
                        S.op("dve", lambda e: e.tensor_tensor(out=eq4, in0=eq4, in1=itb, op=ALU.mult), r=[bigB, it1f], w=[bigB])
                        yield
                        S.op("dve", lambda e: e.tensor_reduce(out=dst[:, q4 * 32:(q4 + 1) * 32], in_=bigB[:, q4 * 512:(q4 + 1) * 512].rearrange("p (a b) -> p a b", b=16),
                                                              axis=AX.X, op=ALU.add), r=[bigB], w=[dst])
                        yield
                if dbg_d and ti == 0 and tsub == 0:
                    sched_barrier(S)
                    for n_, t_ in (("h2Tb", hT), ("qTsb", qTs), ("skTb", skT), ("st1", st1), ("it1f", it1f), ("ts", ts), ("pif", pif), ("pjf", pjf), ("idx1f", idx1f), ("idx2f", idx2f), ("gat", gat)):
                        a_ = t_[:]
                        if len(a_.shape) == 3:
                            a_ = a_.rearrange("p a b -> p (a b)")
                        S.dma("sp", None, dbg_d[n_], t_, a_, final=True)
                    S.dma("sp", None, dbg_d["wqb"], wq, wq[:, 3, :], final=True)
                S.op("act", lambda e: e.activation(out=eg[:], in_=eg[:], func=AF.Exp), r=[eg], w=[eg])
                yield
                S.op("dve", lambda e: e.tensor_reduce(out=sume[:], in_=eg[:], axis=AX.X, op=ALU.add), r=[eg], w=[sume])
                S.op("dve", lambda e: e.reciprocal(out=rse[:], in_=sume[:]), r=[sume], w=[rse])
                S.op("dve", lambda e: e.tensor_tensor(out=gat[:].rearrange("p (h k) -> p h k", h=8), in0=eg[:], in1=rse[:].unsqueeze(2).to_broadcast([128, 8, 16]), op=ALU.mult),
                     r=[eg, rse], w=[gat])
                yield
                p = next_pm()
                for i3, src in enumerate((idx1f, idx2f, gat)):
                    S.op("pe", lambda e, p=p, i3=i3, src=src: e.transpose(out=p[:, i3 * 128:(i3 + 1) * 128], in_=src[:], identity=ident_f[:]), r=[src, ident_f], w=[p], inc=(i3 == 2))
                yield
                for i3, dst in enumerate((idx1T, idx2T, gatT)):
                    S.op("act", lambda e, p=p, i3=i3, dst=dst: e.copy(out=dst[:, tsub * 128:(tsub + 1) * 128], in_=p[:, i3 * 128:(i3 + 1) * 128]), r=[p], w=[dst])
                S.op("act", lambda e, p=p: e.mul(out=nidx2T[:, tsub * 128:(tsub + 1) * 128], in_=p[:, 128:256], mul=-6.0), r=[p], w=[nidx2T])
                S.op("act", lambda e, p=p: e.mul(out=gatTs[:, tsub * 128:(tsub + 1) * 128], in_=p[:, 256:384], mul=1.0 / 1.125), r=[p], w=[gatTs])
                yield

        def back(ti):
            def gen_oh(t4):
                for k4 in range(4):
                    tok = t4 * 4 + k4
                    r_, l_ = Rt[tok % NOH], Lt[tok % NOH]
                    if tok % 2 == 1:
                        S.op("act", lambda e: e.activation(out=r_[:], in_=iota_f[:], func=AF.Derivative_Erf, scale=6.0, bias=nidx2T[:, tok:tok + 1]),
                             r=[iota_f, nidx2T], w=[r_])
                        g_ = gatTs
                    else:
                        S.op("dve", lambda e: e.tensor_scalar(out=r_[:], in0=iota_b[:], scalar1=idx2T[:, tok:tok + 1], scalar2=None, op0=ALU.is_equal),
                             r=[iota_b, idx2T], w=[r_])
                        g_ = gatT
                    S.op("dve", lambda e: e.tensor_scalar(out=l_[:], in0=iota_b[:], scalar1=idx1T[:, tok:tok + 1], scalar2=g_[:, tok:tok + 1], op0=ALU.is_equal, op1=ALU.mult),
                         r=[iota_b, idx1T, g_], w=[l_])

            def gen_mm(t4):
                p = next_g()
                for k4 in range(4):
                    tok = t4 * 4 + k4
                    r_, l_ = Rt[tok % NOH], Lt[tok % NOH]
                    S.op("pe", lambda e: e.matmul(p[:, k4 * 128:(k4 + 1) * 128], lhsT=r_[:], rhs=l_[:], start=True, stop=True), r=[r_, l_], w=[p], inc=(k4 == 3))
                src = cap(p, 0, [[1, 128], [128, 4]])
                S.op("act", lambda e: e.copy(out=G_sb[:, :, t4 * 4:(t4 + 1) * 4], in_=src), r=[p], w=[G_sb])

            for t4 in range(65):
                if t4 < 64:
                    gen_oh(t4)
                if t4 >= 1:
                    gen_mm(t4 - 1)

        def load_group(gg):
            if gg >= NT * NGRP:
                return
            s, g = gg % NSET, gg % NGRP
            S.dma("sp", EX[s], EX[s][:], es_s, es_s[g])

        def a_mm(ti, c):
            s = (ti * NGRP + c // CPG) % NSET
            ci = c % CPG
            ap_ = a_ps[c % 2]
            hT = h2T[ti % 2]
            for kc in range(8):
                S.op("pe", lambda e, kc=kc: e.matmul(ap_[:], lhsT=EX[s][:, kc * 256 + ci * 128:kc * 256 + (ci + 1) * 128], rhs=hT[:, kc, :], start=(kc == 0), stop=(kc == 7)),
                     r=[EX[s], hT], w=[ap_], inc=(kc == 7))
            g_, ga_ = ge[c % 2], GA[c % 3]
            S.op("act", lambda e: e.activation(out=g_[:], in_=ap_[:], func=AF.Gelu), r=[ap_], w=[g_])
            S.op("pool", lambda e: e.tensor_tensor(out=ga_[:], in0=g_[:], in1=G_sb[:, c, :], op=ALU.mult), r=[g_, G_sb], w=[ga_])

        def o_mm(ti, c):
            s = (ti * NGRP + c // CPG) % NSET
            ci = c % CPG
            ga_ = GA[c % 3]
            for tsub in range(2):
                for hf in range(2):
                    last = (tsub == 1 and hf == 1)
                    S.op("pe", lambda e, tsub=tsub, hf=hf: e.matmul(acc[tsub * 2 + hf][:], lhsT=ga_[:, tsub * 128:(tsub + 1) * 128], rhs=EX[s][:, 2048 + ci * 1024 + hf * 512:2048 + ci * 1024 + (hf + 1) * 512],
                                                                   start=(c == 0), stop=(c == 127)), r=[ga_, EX[s]], w=[acc[tsub * 2 + hf]], inc=last)

        LOOKAHEAD = 2
        for gg in range(NSET):
            load_group(gg)
        n_front = sum(1 for _ in front(0))
        for ti in range(NT):
            back(ti)
            fgen = front(ti + 1) if ti + 1 < NT else iter(())
            pulled = 0
            for c in range(128 + LOOKAHEAD):
                if c < 128:
                    a_mm(ti, c)
                if c >= LOOKAHEAD:
                    co = c - LOOKAHEAD
                    o_mm(ti, co)
                    if co % CPG == CPG - 1:
                        load_group(ti * NGRP + co // CPG + NSET)
                if c >= 2:
                    target = ((c - 1) * n_front + 121) // 122
                    while pulled < target:
                        next(fgen, None)
                        pulled += 1
            for _ in fgen:
                pass
            xs = x1t[ti % 2]
            for tsub in range(2):
                for hf in range(2):
                    S.op("dve", lambda e, tsub=tsub, hf=hf: e.tensor_tensor(out=xs[:, tsub, hf * 512:(hf + 1) * 512], in0=acc[tsub * 2 + hf][:], in1=xs[:, tsub, hf * 512:(hf + 1) * 512], op=ALU.add),
                         r=[acc[tsub * 2 + hf], xs], w=[xs])
            S.dma("sp", None, y_d[ti * 256:(ti + 1) * 256, :].rearrange("(b p) d -> p b d", p=128), xs, xs[:], final=True)
        sched_barrier(S)


def _host_layout(inputs):
    f = lambda a: np.ascontiguousarray(np.asarray(a, dtype=np.float32))
    qcols = np.concatenate([np.arange(h * 64, (h + 1) * 64) for h in HEADS_PERM])
    cols = np.arange(4352)
    cols[1536:2048] = 1536 + qcols
    sh = {}
    sh["w_in"] = f(np.asarray(inputs["w_in"])[0][:, cols])
    sh["w_co"] = f(np.asarray(inputs["w_conv_out"])[0])
    sh["w_ao"] = f(np.asarray(inputs["w_attn_out"])[0][qcols, :])
    sh["w_out"] = f(np.asarray(inputs["w_out"])[0])
    sh["w_q"] = f(np.asarray(inputs["w_query"])[0])
    sk = np.asarray(inputs["sub_keys"])[0]
    sh["skT"] = f(sk.transpose(3, 0, 1, 2).reshape(128, 2048))
    eu = np.asarray(inputs["expert_u"])[0]
    sh["utg"] = f(eu.T.reshape(8, 128, NGRP, CPG * 128).transpose(2, 1, 0, 3).reshape(NGRP, 128, 8 * CPG * 128))
    ev = np.asarray(inputs["expert_v"])[0]
    sh["vg"] = f(ev.reshape(NGRP, CPG, 128, 1024).transpose(0, 2, 1, 3).reshape(NGRP, 128, CPG * 1024))
    sh["gmix"] = f(np.tile(np.asarray(inputs["norm_mix"])[0][None, :], (128, 1)))
    sh["gffn"] = f(np.tile(np.asarray(inputs["norm_ffn"])[0][None, :], (128, 1)))
    sh["bgate"] = f(np.asarray(inputs["b_gate"])[0].reshape(16, 128).T)
    sh["wconv"] = f(np.asarray(inputs["w_conv"])[0].reshape(3, 4, 128).transpose(2, 0, 1).reshape(128, 12))
    qn = np.asarray(inputs["q_norm"])[0]
    kn = np.asarray(inputs["k_norm"])[0]
    sh["qkn"] = f(np.stack([np.tile(qn, 2), np.tile(kn, 2)], axis=1))
    sh["sinkr"] = f(np.tile(np.asarray(inputs["sinks"])[0][HEADS_PERM][None, :], (128, 1)))
    sh["relb"] = f(np.asarray(inputs["rel_bias"])[:, HEADS_PERM])
    j = np.arange(384)
    d = j - 127
    valid = (d >= 0) & (d <= 127)
    bk = t5_bucket_np(d)
    ohb = np.zeros((32, 384), np.float32)
    ohb[bk[valid], j[valid]] = 1.0
    sh["ohb"] = ohb
    sh["mask8"] = np.tile(np.where(valid, 0.0, -30000.0).astype(np.float32)[None, :], (8, 1))
    return sh


_PROG = {}


def kernel(**inputs):
    x = np.asarray(inputs["x"], dtype=np.float32)
    B, T, D = x.shape
    if T not in _PROG:
        _PROG[T] = build_program(T)
    nc = _PROG[T]
    sh = _host_layout(inputs)
    in_maps = []
    for b in range(B):
        m = dict(sh)
        m["x"] = np.ascontiguousarray(x[b])
        in_maps.append(m)
    res = run_bass_kernel_spmd(nc, in_maps, core_ids=list(range(B)))
    return np.stack([np.asarray(r["y"]) for r in res.results], axis=0).astype(np.float32)
```
